# Optimizing a Trainium2 kernel written in Bass

```python
import math
import jax, jax.numpy as jnp
from jax import lax
import numpy as np

D_MODEL = 4096
BATCH = 2
SEQ = 8192
DEPTH = 4

N_MIXERS = 2
N_A_LAYERS = (DEPTH + 1) // 2
N_B_LAYERS = DEPTH // 2
CHUNK = 128
A_WIDTH = D_MODEL
A_HEADS = 16
A_HEAD_DIM = A_WIDTH // A_HEADS
B_WIDTH = D_MODEL
B_HEADS = 16
B_HEAD_DIM = B_WIDTH // B_HEADS
CONV_WIDTH = 4
LRU_C = 8.0
N_EXPERTS = 32
TOP_K = 4
EXPERT_FF = (3 * D_MODEL) // 32
SWIGLU_LIMIT = 7.0
SWIGLU_ALPHA = 1.702
MOE_BLOCK = 128
LN_EPS = 1e-5
DEEPNORM_ALPHA = (2 * DEPTH) ** 0.25
DEEPNORM_BETA = (8 * DEPTH) ** -0.25

kernel_name = "hybrid_gmlp_rglru_moe_deepnorm"


def layer_norm(x, g, b):
    xf = x.astype(jnp.float32)
    mu = jnp.mean(xf, axis=-1, keepdims=True)
    xc = xf - mu
    var = jnp.mean(xc * xc, axis=-1, keepdims=True)
    y = xc * lax.rsqrt(var + LN_EPS) * g.astype(jnp.float32) + b.astype(jnp.float32)
    return y.astype(x.dtype)


def chunked_gmlp(x, w_in, ln_g, ln_b, w_s, b_s, w_out):
    bsz, seq, _ = x.shape
    z = jax.nn.gelu(x @ w_in)
    u, v = jnp.split(z, 2, axis=-1)
    v = layer_norm(v, ln_g, ln_b)
    v = v.reshape(bsz, seq // CHUNK, CHUNK, A_HEADS, A_HEAD_DIM)
    causal = jnp.tril(jnp.ones((CHUNK, CHUNK), dtype=w_s.dtype))
    w = w_s * causal
    sv = jnp.einsum('hts,bcshd->bcthd', w, v) + b_s.T[None, None, :, :, None]
    sv = sv.reshape(bsz, seq, A_WIDTH)
    return (u * sv) @ w_out


def _lru_combine(left, right):
    a1, b1 = left
    a2, b2 = right
    return a1 * a2, a2 * b1 + b2


def rglru_block(x, w_in, conv_w, conv_b, w_r, b_r, w_i, b_i, lam, w_out):
    bsz, seq, _ = x.shape
    y, xr = jnp.split(x @ w_in, 2, axis=-1)
    xp = jnp.pad(xr, ((0, 0), (CONV_WIDTH - 1, 0), (0, 0)))
    xc = conv_b
    for k in range(CONV_WIDTH):
        xc = xc + xp[:, k:k + seq] * conv_w[k]
    xh = xc.reshape(bsz, seq, B_HEADS, B_HEAD_DIM)
    r = jax.nn.sigmoid(jnp.einsum('bshd,hde->bshe', xh, w_r) + b_r).reshape(bsz, seq, B_WIDTH)
    i = jax.nn.sigmoid(jnp.einsum('bshd,hde->bshe', xh, w_i) + b_i).reshape(bsz, seq, B_WIDTH)
    log_a = (-LRU_C * r.astype(jnp.float32)) * jax.nn.softplus(-lam.astype(jnp.float32))
    a = jnp.exp(log_a)
    mult = jnp.sqrt(-jnp.expm1(2.0 * log_a))
    bterm = mult * (i * xc).astype(jnp.float32)
    _, h = lax.associative_scan(_lru_combine, (a, bterm), axis=1)
    return (h.astype(x.dtype) * jax.nn.gelu(y)) @ w_out


def moe(x, router_w, router_b, w_gu, b_gu, w_down, b_down):
    bsz, seq, d = x.shape
    n_tok = bsz * seq
    x2d = x.reshape(n_tok, d)
    logits = x2d.astype(jnp.float32) @ router_w.astype(jnp.float32) + router_b.astype(jnp.float32)
    top_logit, top_e = lax.top_k(logits, TOP_K)
    gate = jax.nn.softmax(top_logit, axis=-1)
    n_assign = n_tok * TOP_K
    flat_e = top_e.reshape(n_assign).astype(jnp.int32)
    flat_tok = jnp.arange(n_assign, dtype=jnp.int32) // TOP_K
    flat_gate = gate.reshape(n_assign)
    order = jnp.argsort(flat_e)
    sorted_e = flat_e[order]
    counts = jnp.bincount(flat_e, length=N_EXPERTS)
    padded = (counts + MOE_BLOCK - 1) // MOE_BLOCK * MOE_BLOCK
    padded_end = jnp.cumsum(padded)
    padded_start = padded_end - padded
    start = jnp.cumsum(counts) - counts
    dest = padded_start[sorted_e] + jnp.arange(n_assign, dtype=jnp.int32) - start[sorted_e]
    n_blocks = -(-(n_assign + N_EXPERTS * (MOE_BLOCK - 1)) // MOE_BLOCK)
    n_rows = n_blocks * MOE_BLOCK
    row_tok = jnp.zeros((n_rows,), jnp.int32).at[dest].set(flat_tok[order])
    row_gate = jnp.zeros((n_rows,), jnp.float32).at[dest].set(flat_gate[order])
    block_start = jnp.arange(n_blocks, dtype=jnp.int32) * MOE_BLOCK
    block_e = jnp.minimum(jnp.searchsorted(padded_end, block_start, side='right'), N_EXPERTS - 1)

    def block_step(acc, blk):
        rows, g, e = blk
        xb = x2d[rows]
        gu = xb @ w_gu[e] + b_gu[e]
        hg, hu = gu[:, :EXPERT_FF], gu[:, EXPERT_FF:]
        hg = jnp.minimum(hg, SWIGLU_LIMIT)
        hu = jnp.clip(hu, -SWIGLU_LIMIT, SWIGLU_LIMIT)
        hdn = hg * jax.nn.sigmoid(SWIGLU_ALPHA * hg) * (hu + 1.0)
        yb = hdn @ w_down[e] + b_down[e]
        acc = acc.at[rows].add(yb.astype(jnp.float32) * g[:, None])
        return acc, None

    acc0 = jnp.zeros((n_tok, d), jnp.float32)
    out, _ = lax.scan(block_step, acc0,
                      (row_tok.reshape(n_blocks, MOE_BLOCK), row_gate.reshape(n_blocks, MOE_BLOCK), block_e))
    return out.astype(x.dtype).reshape(bsz, seq, d)


def setup_inputs(seed: int = 0) -> dict:
    key = jax.random.key(seed)
    ks = jax.random.split(key, 32)
    f32 = jnp.float32
    nrm = lambda k, shape, s: jax.random.normal(k, shape, f32) * s
    nA, nB = N_A_LAYERS, N_B_LAYERS
    u = jax.random.uniform(ks[13], (nB, B_WIDTH), f32, 0.9, 0.999)
    p = u ** (1.0 / LRU_C)
    lam = jnp.log(p) - jnp.log1p(-p)
    return {
        "x": jax.random.normal(ks[0], (BATCH, SEQ, D_MODEL), f32),
        "a_w_in": nrm(ks[1], (nA, D_MODEL, 2 * A_WIDTH), D_MODEL ** -0.5),
        "a_ln_g": 1.0 + nrm(ks[2], (nA, A_WIDTH), 0.02),
        "a_ln_b": nrm(ks[3], (nA, A_WIDTH), 0.02),
        "a_w_s": nrm(ks[4], (nA, A_HEADS, CHUNK, CHUNK), CHUNK ** -0.5),
        "a_b_s": 1.0 + nrm(ks[5], (nA, A_HEADS, CHUNK), 0.02),
        "a_w_out": nrm(ks[6], (nA, A_WIDTH, D_MODEL), A_WIDTH ** -0.5 * DEEPNORM_BETA),
        "b_w_in": nrm(ks[7], (nB, D_MODEL, 2 * B_WIDTH), D_MODEL ** -0.5),
        "b_conv_w": nrm(ks[8], (nB, CONV_WIDTH, B_WIDTH), CONV_WIDTH ** -0.5),
        "b_conv_b": nrm(ks[9], (nB, B_WIDTH), 0.02),
        "b_w_r": nrm(ks[10], (nB, B_HEADS, B_HEAD_DIM, B_HEAD_DIM), B_HEAD_DIM ** -0.5),
        "b_b_r": nrm(ks[11], (nB, B_HEADS, B_HEAD_DIM), 0.02),
        "b_w_i": nrm(ks[12], (nB, B_HEADS, B_HEAD_DIM, B_HEAD_DIM), B_HEAD_DIM ** -0.5),
        "b_b_i": nrm(ks[14], (nB, B_HEADS, B_HEAD_DIM), 0.02),
        "b_lambda": lam,
        "b_w_out": nrm(ks[15], (nB, B_WIDTH, D_MODEL), B_WIDTH ** -0.5 * DEEPNORM_BETA),
        "ln1_g": 1.0 + nrm(ks[16], (DEPTH, D_MODEL), 0.02),
        "ln1_b": nrm(ks[17], (DEPTH, D_MODEL), 0.02),
        "ln2_g": 1.0 + nrm(ks[18], (DEPTH, D_MODEL), 0.02),
        "ln2_b": nrm(ks[19], (DEPTH, D_MODEL), 0.02),
        "router_w": nrm(ks[20], (DEPTH, D_MODEL, N_EXPERTS), D_MODEL ** -0.5),
        "router_b": nrm(ks[21], (DEPTH, N_EXPERTS), 0.01),
        "ex_w_gu": nrm(ks[22], (DEPTH, N_EXPERTS, D_MODEL, 2 * EXPERT_FF), D_MODEL ** -0.5),
        "ex_b_gu": nrm(ks[23], (DEPTH, N_EXPERTS, 2 * EXPERT_FF), 0.02),
        "ex_w_down": nrm(ks[24], (DEPTH, N_EXPERTS, EXPERT_FF, D_MODEL), EXPERT_FF ** -0.5 * DEEPNORM_BETA),
        "ex_b_down": nrm(ks[25], (DEPTH, N_EXPERTS, D_MODEL), 0.02),
    }


def reference(x, a_w_in, a_ln_g, a_ln_b, a_w_s, a_b_s, a_w_out,
              b_w_in, b_conv_w, b_conv_b, b_w_r, b_b_r, b_w_i, b_b_i, b_lambda, b_w_out,
              ln1_g, ln1_b, ln2_g, ln2_b,
              router_w, router_b, ex_w_gu, ex_b_gu, ex_w_down, ex_b_down):
    h = x
    for layer in range(DEPTH):
        j = layer // N_MIXERS
        if layer % N_MIXERS == 0:
            mix = chunked_gmlp(h, a_w_in[j], a_ln_g[j], a_ln_b[j], a_w_s[j], a_b_s[j], a_w_out[j])
        else:
            mix = rglru_block(h, b_w_in[j], b_conv_w[j], b_conv_b[j], b_w_r[j], b_b_r[j],
                              b_w_i[j], b_b_i[j], b_lambda[j], b_w_out[j])
        h = layer_norm(DEEPNORM_ALPHA * h + mix, ln1_g[layer], ln1_b[layer])
        ffn = moe(h, router_w[layer], router_b[layer], ex_w_gu[layer], ex_b_gu[layer],
                  ex_w_down[layer], ex_b_down[layer])
        h = layer_norm(DEEPNORM_ALPHA * h + ffn, ln2_g[layer], ln2_b[layer])
    return h
```

```python
import math
import numpy as np
from contextlib import ExitStack
import concourse.bass as bass
import concourse.mybir as mybir
from concourse.bass_utils import run_bass_kernel_spmd

F32 = mybir.dt.float32
BF16 = mybir.dt.bfloat16
AF = mybir.ActivationFunctionType
ALU = mybir.AluOpType
AX = mybir.AxisListType

ENGS = ["sync", "scalar", "vector", "gpsimd", "tensor"]
SEM_LIMIT = 30000


class Cfg:
    def __init__(self, D=4096, NTOK=8192, DEPTH=4, AH=16, BH=16, E=32, F=384, TOPK=4, n_cores=2):
        self.D = D
        self.KC = D // 128
        self.NTOK = NTOK
        self.TT = 512
        self.NST = NTOK // 512
        self.DEPTH = DEPTH
        self.AH = AH
        self.BH = BH
        self.E = E
        self.F = F
        self.FC = F // 128
        self.TOPK = TOPK
        self.n_cores = n_cores
        self.nA = (DEPTH + 1) // 2
        self.nB = DEPTH // 2
        self.ALPHA = float((2 * DEPTH) ** 0.25)
        self.EPS = 1e-5
        self.EG = min(8, E)
        self.NG = E // self.EG


class Ev:
    __slots__ = ("sem", "val")

    def __init__(self, sem, val):
        self.sem = sem
        self.val = val


def _flat(deps):
    out = []
    if deps is None:
        return out
    if isinstance(deps, Ev):
        return [deps]
    for d in deps:
        if d is None:
            continue
        if isinstance(d, Ev):
            out.append(d)
        else:
            out.extend(_flat(d))
    return out


class Prog:
    def __init__(self, nc, stack):
        self.nc = nc
        self.stack = stack
        self.q = {e: [] for e in ENGS}
        self.cur = {}
        self.waited = {e: {} for e in ENGS}
        self.nsem = 0
        self.ninst = 0

    def new_sem(self, name):
        self.nsem += 1
        return self.stack.enter_context(self.nc.semaphore(f"{name}_{self.nsem}"))

    def _eng_sem(self, eng):
        c = self.cur.get(eng)
        if c is None or c[1] >= SEM_LIMIT:
            c = [self.new_sem("e" + eng), 0]
            self.cur[eng] = c
        return c

    def _waits(self, eng, deps):
        wd = self.waited[eng]
        best = {}
        for d in _flat(deps):
            k = id(d.sem)
            if wd.get(k, 0) >= d.val:
                continue
            if k not in best or best[k][1] < d.val:
                best[k] = (d.sem, d.val)
        w = []
        for k, (sem, val) in best.items():
            wd[k] = val
            w.append((sem, val))
        return w

    def op(self, eng, fn, deps=(), signal=True):
        waits = self._waits(eng, deps)
        ev = None
        inc = None
        if signal:
            c = self._eng_sem(eng)
            c[1] += 1
            ev = Ev(c[0], c[1])
            inc = (c[0], 1)
        self.q[eng].append((fn, waits, inc))
        self.ninst += 1
        return ev

    def dma(self, eng, fn, sem_state, deps=()):
        waits = self._waits(eng, deps)
        sem_state[1] += 16
        ev = Ev(sem_state[0], sem_state[1])
        self.q[eng].append((fn, waits, (sem_state[0], 16)))
        self.ninst += 1
        return ev

    def dsem(self, name="d"):
        return [self.new_sem(name), 0]

    def wait_only(self, eng, deps):
        waits = self._waits(eng, deps)
        if waits:
            self.q[eng].append((None, waits, None))

    def replay(self, block):
        for eng in ENGS:
            items = self.q[eng]
            if not items:
                continue

            def body(e, items=items):
                for fn, waits, inc in items:
                    for (s, v) in waits:
                        e.wait_ge(s, v)
                    if fn is None:
                        continue
                    ins = fn(e)
                    if inc is not None:
                        ins.then_inc(inc[0], inc[1])

            getattr(block, eng)(body)


class Buf:
    def __init__(self, ap):
        self.ap = ap
        self.ready = []
        self.free = []


class Ring:
    def __init__(self, aps):
        self.bufs = [Buf(a) for a in aps]
        self.i = 0

    def get(self):
        b = self.bufs[self.i % len(self.bufs)]
        self.i += 1
        return b


def build_program(cfg, layers=None, upto=99):
    c = cfg
    D, KC, TT, NST, E, FC = c.D, c.KC, c.TT, c.NST, c.E, c.FC
    layers = list(range(c.DEPTH)) if layers is None else layers
    nc = bass.Bass("TRN2", target_bir_lowering=False)

    def din(name, shape):
        return nc.dram_tensor(name, list(shape), F32, kind="ExternalInput").ap()

    nA1, nB1 = max(c.nA, 1), max(c.nB, 1)
    x = din("x", [c.NTOK, D])
    a_w_in = din("a_w_in", [c.nA, D, 2 * D])
    a_w_out = din("a_w_out", [c.nA, D, D])
    b_w_in = din("b_w_in", [c.nB, D, 2 * D])
    b_w_r = din("b_w_r", [c.nB, c.BH, 256, 256])
    b_w_i = din("b_w_i", [c.nB, c.BH, 256, 256])
    b_w_out = din("b_w_out", [c.nB, D, D])
    ln1_g = din("ln1_g", [c.DEPTH, D])
    ln1_b = din("ln1_b", [c.DEPTH, D])
    ln2_g = din("ln2_g", [c.DEPTH, D])
    ln2_b = din("ln2_b", [c.DEPTH, D])
    router_b = din("router_b", [c.DEPTH, E])
    ex_w_gu = din("ex_w_gu", [c.DEPTH, E, D, 2 * c.F])
    ex_w_down = din("ex_w_down", [c.DEPTH, E, c.F, D])
    ex_b_down = din("ex_b_down", [c.DEPTH, E, D])
    a_b_s = din("a_b_s", [c.nA, c.AH, 128])
    l_a_ln_g = din("l_a_ln_g", [128, nA1 * KC])
    l_a_ln_b = din("l_a_ln_b", [128, nA1 * KC])
    l_a_w_sT = din("l_a_w_sT", [nA1, 128, c.AH * 128])
    l_conv_w = din("l_conv_w", [128, nB1 * 4 * KC])
    l_conv_b = din("l_conv_b", [128, nB1 * KC])
    l_b_r = din("l_b_r", [128, nB1 * KC])
    l_b_i = din("l_b_i", [128, nB1 * KC])
    l_lam = din("l_lam", [128, nB1 * KC])
    l_router_w = din("l_router_w", [c.DEPTH, 128, KC * E])
    l_b_gu = din("l_b_gu", [c.DEPTH, 128, E * 2 * FC])
    c_ident = din("c_ident", [128, 128])
    c_triu = din("c_triu", [128, 128])
    out = nc.dram_tensor("out", [c.NTOK, D], F32, kind="ExternalOutput").ap()
    big = {"a_w_in": a_w_in, "a_w_out": a_w_out, "b_w_in": b_w_in, "b_w_out": b_w_out,
           "ex_w_gu": ex_w_gu, "ex_w_down": ex_w_down}
    bigb = {}
    for nm_, ap_ in big.items():
        bigb[nm_] = [nc.dram_tensor(f"{nm_}_bf{i_}", list(ap_.shape[1:]), BF16).ap() for i_ in range(ap_.shape[0])]
    wmt_d = nc.dram_tensor("wmt_d", [nA1, 128, c.AH * 128], BF16).ap()
    bt_d = nc.dram_tensor("bt_d", [nA1, 128, KC * 128], F32).ap()

    st = ExitStack()
    with st:
        P = Prog(nc, st)

        def sb(name, shape, dt):
            return st.enter_context(nc.sbuf_tensor(name, list(shape), dt))

        ACC = sb("ACC", [128, 4, D], F32)
        XT = sb("XT", [128, KC * TT], BF16)
        UT = sb("UT", [128, max(KC * TT, c.EG * FC * TT)], BF16)
        VB = sb("VB", [128, 8192], F32)
        WR = [sb(f"WR{i}", [128, max(KC, c.EG * FC) * 256], BF16) for i in range(2)]
        ident = sb("ident", [128, 128], F32)
        XT3 = XT[:].rearrange("p (k t) -> p k t", k=KC)
        UT3 = UT[:, 0:KC * TT].rearrange("p (k t) -> p k t", k=KC)
        VBb = VB[:].bitcast(BF16)

        lng = sb("lng", [128, nA1 * KC], F32)
        lnb = sb("lnb", [128, nA1 * KC], F32)
        cw = sb("cw", [128, nB1 * 4 * KC], F32)
        cb_ = sb("cb", [128, nB1 * KC], F32)
        bbr = sb("bbr", [128, nB1 * KC], F32)
        bbi = sb("bbi", [128, nB1 * KC], F32)
        c8 = sb("c8", [128, nB1 * KC], F32)
        hstate = sb("hstate", [128, nB1 * KC], F32)
        halo = sb("halo", [128, nB1 * KC * 3], F32)
        small = sb("small", [128, 64], F32)
        epsc = sb("epsc", [128, 1], F32)
        lnst = sb("lnst", [128, 4 * 96], F32)
        sp_tmp_t = sb("sp_tmp", [128, 2 * 128], F32)
        gw_t0 = sb("gwt", [128, 2 * 1024], BF16)

        wring = Ring([w[:] for w in WR])
        wsem = [P.dsem("w0"), P.dsem("w1")]
        PSB = [st.enter_context(nc.psum_tensor(f"ps{i}", [128, 512], F32)) for i in range(8)]
        psring = Ring([p[:] for p in PSB[0:7]])
        lgbank = Buf(PSB[7][:])

        acc_b = [Buf(ACC[:, tt, :]) for tt in range(4)]
        xt_b = Buf(XT3)
        ut_b = Buf(UT3)
        vb_b = Buf(VB[:])
        sem_in = [P.dsem(f"in{i}") for i in range(4)]
        sem_out = [P.dsem(f"out{i}") for i in range(4)]
        sem_c = P.dsem("const")
        sem_pr = P.dsem("prep")
        sem_p = P.dsem("par")
        sem_p2 = P.dsem("par2")
        sem_p3 = P.dsem("par3")
        bsem = [P.dsem("bd0"), P.dsem("bd1")]
        sp_tmp = Ring([sp_tmp_t[:, 0:128], sp_tmp_t[:, 128:256]])
        gw_r = Ring([gw_t0[:, 0:1024], gw_t0[:, 1024:2048]])
        for i_, b_ in enumerate(gw_r.bufs):
            b_.sem = P.dsem(f"gw{i_}")
        vbfree = [[], []]

        def wload(src3, nk, ncols):
            slot = wring.i % 2
            b = wring.get()
            view = b.ap[:, 0:nk * ncols].rearrange("p (k n) -> p k n", k=nk)
            ev = P.dma("gpsimd", lambda e, view=view, src3=src3: e.dma_start(out=view, in_=src3), wsem[slot], deps=b.free + cvt_all)
            b.free = []
            b.ready = [ev]
            return b, view

        def psget():
            return psring.get()

        sem_cv = P.dsem("cvt")
        cvt_ev = []
        CH = 8192
        for nm_ in ["a_w_in", "b_w_in", "a_w_out", "b_w_out", "ex_w_gu", "ex_w_down"]:
            for li_ in range(big[nm_].shape[0]):
                src_, dst_ = big[nm_][li_], bigb[nm_][li_]
                tot = int(np.prod(src_.shape))
                names = " ".join(f"d{i}" for i in range(len(src_.shape)))
                sf = src_.rearrange(f"{names} -> ({names})").rearrange("(p n) -> p n", p=128)
                df = dst_.rearrange(f"{names} -> ({names})").rearrange("(p n) -> p n", p=128)
                per = tot // 128
                for o in range(0, per, CH):
                    w_ = min(CH, per - o)
                    cvt_ev.append(P.dma("gpsimd", lambda e, sf=sf, df=df, o=o, w_=w_: e.dma_start(out=df[:, o:o + w_], in_=sf[:, o:o + w_]),
                                        sem_cv))
        cvt_all = cvt_ev[-1:]
        a_w_in, a_w_out, b_w_in, b_w_out, ex_w_gu, ex_w_down = (bigb[k_] for k_ in
                                                                  ["a_w_in", "a_w_out", "b_w_in", "b_w_out", "ex_w_gu", "ex_w_down"])

        cev = [P.dma("sync", lambda e: e.dma_start(out=ident[:], in_=c_ident[:, :]), sem_c)]

        def cload(dst, src):
            cev.append(P.dma("sync", lambda e: e.dma_start(out=dst, in_=src), sem_c))

        cload(lng[:], l_a_ln_g[:, :])
        cload(lnb[:], l_a_ln_b[:, :])
        cload(cw[:], l_conv_w[:, :])
        cload(cb_[:], l_conv_b[:, :])
        cload(bbr[:], l_b_r[:, :])
        cload(bbi[:], l_b_i[:, :])
        cload(c8[:], l_lam[:, :])
        const_ev = list(cev)
        e1 = P.op("scalar", lambda e: e.activation(out=c8[:], in_=c8[:], func=AF.Exp, scale=-1.0), deps=cev)
        e2 = P.op("scalar", lambda e: e.activation(out=c8[:], in_=c8[:], func=AF.Ln, bias=1.0), deps=[e1])
        e3 = P.op("scalar", lambda e: e.mul(out=c8[:], in_=c8[:], mul=-8.0), deps=[e2])
        e4 = P.op("vector", lambda e: e.memset(hstate[:], 0.0))
        e5 = P.op("vector", lambda e: e.memset(halo[:], 0.0), deps=[e4])
        e6 = P.op("vector", lambda e: e.memset(epsc[:], c.EPS), deps=[e5])
        const_ev += [e3, e4, e5, e6]

        prep_ev = []
        AHn = c.AH
        for l in range(c.nA):
            WT32 = ACC[:, 0, 0:AHn * 128].rearrange("p (h t) -> p h t", h=AHn)
            WmTb = VBb[:, 0:AHn * 128].rearrange("p (h t) -> p h t", h=AHn)
            bsbc = ACC[:, 1, 0:AHn * 128].rearrange("p (h t) -> p h t", h=AHn)
            BTt = ACC[:, 2, 0:KC * 128].rearrange("p (j t) -> p j t", j=KC)
            triu = VB[:, 4096:4096 + 128]
            ones = VB[:, 4096 + 128:4096 + 256]
            d0 = list(prep_ev)
            l1 = P.dma("sync", lambda e, l=l: e.dma_start(out=ACC[:, 0, 0:AHn * 128], in_=l_a_w_sT[l]), sem_pr, deps=d0)
            l2 = P.dma("sync", lambda e, triu=triu: e.dma_start(out=triu, in_=c_triu[:, :]), sem_pr, deps=d0)
            l3 = P.dma("sync", lambda e, l=l: e.dma_start(out=ACC[:, 1, 0:AHn * 128],
                                                          in_=a_b_s[l].rearrange("h t -> (h t)").partition_broadcast(128)),
                       sem_pr, deps=d0)
            m0 = P.op("vector", lambda e, ones=ones: e.memset(ones, 1.0), deps=d0 + const_ev)
            mk = m0
            for h in range(AHn):
                mk = P.op("vector", lambda e, h=h, WT32=WT32, triu=triu: e.tensor_tensor(out=WT32[:, h, :], in0=WT32[:, h, :], in1=triu, op=ALU.mult),
                          deps=[l1, l2, l3, mk])
            cvt = P.op("vector", lambda e, WT32=WT32, WmTb=WmTb: e.tensor_copy(out=WmTb, in_=WT32), deps=[mk])
            bev = []
            for q in range((AHn * 128 + 511) // 512):
                pb = psget()
                ncol = min(512, AHn * 128 - q * 512)
                mm = P.op("tensor", lambda e, q=q, pb=pb, ncol=ncol, ones=ones: e.matmul(
                    pb.ap[:, 0:ncol], lhsT=ones, rhs=ACC[:, 0, q * 512:q * 512 + ncol], start=True, stop=True),
                    deps=[mk, m0] + pb.free)
                rl = []
                for hh in range(ncol // 128):
                    h = q * 4 + hh
                    for jj in range(2):
                        j = h * 2 + jj
                        rl.append(P.op("vector", lambda e, pb=pb, hh=hh, j=j, h=h, l=l, BTt=BTt, bsbc=bsbc: e.scalar_tensor_tensor(
                            out=BTt[:, j, :], in0=pb.ap[:, hh * 128:(hh + 1) * 128], scalar=lnb[:, l * KC + j:l * KC + j + 1],
                            in1=bsbc[:, h, :], op0=ALU.mult, op1=ALU.add), deps=[mm, l3] + const_ev))
                pb.free = rl
                bev += rl
            s1 = P.dma("sync", lambda e, l=l: e.dma_start(out=wmt_d[l], in_=VBb[:, 0:AHn * 128]), sem_pr, deps=[cvt])
            s2 = P.dma("sync", lambda e, l=l: e.dma_start(out=bt_d[l], in_=ACC[:, 2, 0:KC * 128]), sem_pr, deps=bev)
            prep_ev = [s1, s2]
        for b in acc_b:
            b.free = list(prep_ev)
        vb_b.free = list(prep_ev)
        allconst = const_ev + prep_ev

        def build_xt(router_layer=None):
            evs_all = []
            rd = []
            if router_layer is not None:
                L = router_layer
                rw = VB[:, 0:KC * E].rearrange("p (k e) -> p k e", k=KC)
                rwev = P.dma("sync", lambda e: e.dma_start(out=VB[:, 0:KC * E], in_=l_router_w[L]), sem_p, deps=vb_b.free)
                stg_r = Ring([VB[:, 2048:2560], VB[:, 2560:3072]])
                for sgb in stg_r.bufs:
                    sgb.free = list(vb_b.free)
            for tt in range(4):
                for q in range(KC // 4):
                    pb = psget()
                    tl = None
                    for i in range(4):
                        kc = q * 4 + i
                        tl = P.op("tensor", lambda e, tt=tt, kc=kc, i=i, pb=pb: e.transpose(
                            out=pb.ap[:, i * 128:(i + 1) * 128], in_=ACC[:, tt, kc * 128:(kc + 1) * 128], identity=ident[:]),
                            deps=(acc_b[tt].ready + pb.free + allconst) if i == 0 else (), signal=(i == 3))
                    ce = P.op("scalar", lambda e, tt=tt, q=q, pb=pb: e.copy(
                        out=XT3[:, q * 4:(q + 1) * 4, tt * 128:(tt + 1) * 128],
                        in_=pb.ap.rearrange("p (k t) -> p k t", k=4)), deps=[tl] + xt_b.free)
                    fr = [ce]
                    if router_layer is not None:
                        sg = stg_r.get()
                        c2 = P.op("vector", lambda e, pb=pb, sg=sg: e.tensor_copy(out=sg.ap, in_=pb.ap), deps=[tl, ce] + sg.free)
                        fr.append(c2)
                        ml = None
                        for i in range(4):
                            kc = q * 4 + i
                            ml = P.op("tensor", lambda e, i=i, kc=kc, sg=sg, tt=tt, rw=rw: e.matmul(
                                lgbank.ap[:, tt * E:(tt + 1) * E], lhsT=sg.ap[:, i * 128:(i + 1) * 128], rhs=rw[:, kc, :],
                                start=(kc == 0), stop=(kc == KC - 1)),
                                deps=([c2, rwev] + (lgbank.free if (kc == 0 and tt == 0) else [])) if i == 0 else (), signal=(i == 3))
                        sg.free = [ml]
                        rd.append(ml)
                    pb.free = fr
                    evs_all.append(ce)
                    rd.append(tl)
            xt_b.free = []
            xt_b.ready = evs_all[-1:]
            for tt in range(4):
                acc_b[tt].free = acc_b[tt].free + rd[-2:]
            return rd[-2:]

        def layer_norm(gsrc, bsrc):
            g2 = VB[0:KC, 7168:7168 + 128]
            b2 = VB[0:KC, 7168 + 128:7168 + 256]
            import os as _os
            _dbg = int(_os.environ.get("LNDBG", "9"))
            pg = pb_ = None
            if _dbg != 0:
                pg = P.dma("sync", lambda e: e.dma_start(out=g2, in_=gsrc.rearrange("(k c) -> k c", c=128)), sem_p2, deps=vb_b.free)
                pb_ = P.dma("sync", lambda e: e.dma_start(out=b2, in_=bsrc.rearrange("(k c) -> k c", c=128)), sem_p2, deps=vb_b.free)
            nblk = D // 512
            norm_ev = []
            for tt in range(4):
                sv = lnst[:, tt * 96:(tt + 1) * 96]
                se = None
                _lnv = int(_os.environ.get("LNV", "0"))
                for blk in range(nblk):
                    if _lnv == 2:
                        se = P.op("vector", lambda e, tt=tt, blk=blk, sv=sv: e.tensor_copy(
                            out=sv[:, blk * 6:(blk + 1) * 6], in_=ACC[:, tt, blk * 512:blk * 512 + 6]),
                            deps=acc_b[tt].ready if blk == 0 else (), signal=(blk == nblk - 1))
                        continue
                    se = P.op("vector", lambda e, tt=tt, blk=blk, sv=sv: e.bn_stats(
                        out=sv[:, blk * 6:(blk + 1) * 6], in_=ACC[:, tt, blk * 512:(blk + 1) * 512]),
                        deps=(acc_b[tt].ready if _lnv != 1 else ()) if blk == 0 else (), signal=(blk == nblk - 1))
                mv = small[:, tt * 4:tt * 4 + 2]
                rs = small[:, tt * 4 + 2:tt * 4 + 3]
                _sub = int(_os.environ.get("LNSUB", "9"))
                if _sub < 2:
                    norm_ev.append(se); continue
                a1 = P.op("vector", lambda e, sv=sv, mv=mv: e.bn_aggr(out=mv, in_=sv[:, 0:nblk * 6]), deps=[se])
                if _sub < 3:
                    norm_ev.append(a1); continue
                a2a = P.op("scalar", lambda e, mv=mv, rs=rs: e.activation(out=rs, in_=mv[:, 1:2], func=AF.Sqrt, bias=epsc[:, 0:1]), deps=[a1] + allconst)
                if _sub < 4:
                    norm_ev.append(a2a); continue
                a2 = P.op("vector", lambda e, rs=rs: e.reciprocal(out=rs, in_=rs), deps=[a2a])
                if _sub < 5:
                    norm_ev.append(a2); continue
                a3 = P.op("vector", lambda e, tt=tt, mv=mv, rs=rs: e.tensor_scalar(
                    out=ACC[:, tt, :], in0=ACC[:, tt, :], scalar1=mv[:, 0:1], scalar2=rs, op0=ALU.subtract, op1=ALU.mult),
                    deps=[a2] + acc_b[tt].free)
                norm_ev.append(a3)
            fin = None
            mg = mb = None
            import os as _os
            _dbg = int(_os.environ.get("LNDBG", "9"))
            if _dbg < 2:
                for tt in range(4):
                    acc_b[tt].ready = [norm_ev[-1]]
                    acc_b[tt].free = []
                vb_b.free = [pg, pb_]
                return
            for blk in range(nblk):
                bg = psget()
                bb = psget()
                for qd in range(4):
                    kc = blk * 4 + qd
                    sel = ident[0:KC, kc:kc + 1].to_broadcast([KC, 128])
                    mg = P.op("tensor", lambda e, qd=qd, sel=sel, bg=bg: e.matmul(bg.ap[:, qd * 128:(qd + 1) * 128], lhsT=sel, rhs=g2,
                                                                                 start=True, stop=True),
                              deps=([pg, pb_] + bg.free + allconst) if qd == 0 else (), signal=(qd == 3))
                for qd in range(4):
                    kc = blk * 4 + qd
                    sel = ident[0:KC, kc:kc + 1].to_broadcast([KC, 128])
                    mb = P.op("tensor", lambda e, qd=qd, sel=sel, bb=bb: e.matmul(bb.ap[:, qd * 128:(qd + 1) * 128], lhsT=sel, rhs=b2,
                                                                                 start=True, stop=True),
                              deps=([pg, pb_] + bb.free) if qd == 0 else (), signal=(qd == 3))
                l1 = l2 = None
                if _dbg < 3:
                    bg.free = [mg]
                    bb.free = [mb]
                    fin = norm_ev[-1]
                    continue
                for tt in range(4):
                    l1 = P.op("vector", lambda e, tt=tt, blk=blk, bg=bg: e.tensor_tensor(
                        out=ACC[:, tt, blk * 512:(blk + 1) * 512], in0=ACC[:, tt, blk * 512:(blk + 1) * 512], in1=bg.ap, op=ALU.mult),
                        deps=[mg, norm_ev[tt]])
                    l2 = P.op("vector", lambda e, tt=tt, blk=blk, bb=bb: e.tensor_tensor(
                        out=ACC[:, tt, blk * 512:(blk + 1) * 512], in0=ACC[:, tt, blk * 512:(blk + 1) * 512], in1=bb.ap, op=ALU.add),
                        deps=[mb, l1])
                    fin = l2
                bg.free = [l1]
                bb.free = [l2]
            for tt in range(4):
                acc_b[tt].ready = [fin]
                acc_b[tt].free = []
            vb_b.free = [mg, mb]

        def wout_phase(w_out_l, ut_ready):
            fin = None
            mm = None
            wsrc = w_out_l.rearrange("(k p) n -> p k n", p=128)
            nkh = KC // 2
            for cbk in range(D // 512):
                banks = [psget() for _ in range(4)]
                for half in range(2):
                    wb, wv = wload(wsrc[:, half * nkh:(half + 1) * nkh, cbk * 512:(cbk + 1) * 512], nkh, 512)
                    for tt in range(4):
                        pb = banks[tt]
                        for k in range(nkh):
                            kc = half * nkh + k
                            mm = P.op("tensor", lambda e, tt=tt, kc=kc, k=k, pb=pb, wv=wv: e.matmul(
                                pb.ap[:, 0:512], lhsT=UT3[:, kc, tt * 128:(tt + 1) * 128], rhs=wv[:, k, :],
                                start=(kc == 0), stop=(kc == KC - 1)),
                                deps=(wb.ready + (pb.free if half == 0 else []) + ut_ready) if k == 0 else (), signal=(k == nkh - 1))
                    wb.free = [mm]
                for tt in range(4):
                    pb = banks[tt]
                    ev = P.op("vector", lambda e, tt=tt, cbk=cbk, pb=pb: e.scalar_tensor_tensor(
                        out=ACC[:, tt, cbk * 512:(cbk + 1) * 512], in0=ACC[:, tt, cbk * 512:(cbk + 1) * 512], scalar=c.ALPHA,
                        in1=pb.ap[:, 0:512], op0=ALU.mult, op1=ALU.add), deps=[mm] + acc_b[tt].free + acc_b[tt].ready)
                    pb.free = [ev]
                    fin = ev
            for tt in range(4):
                acc_b[tt].ready = [fin]
                acc_b[tt].free = []
            ut_b.free = [mm]

        def mixer_a(l):
            w_in = a_w_in[l].rearrange("(k p) n -> p k n", p=128)
            V3 = VBb.rearrange("p (t d) -> p t d", t=4)[:, :, 0:D]
            u_last = None
            mm = None
            for cbk in range(D // 256):
                wb, wv = wload(w_in[:, :, cbk * 256:(cbk + 1) * 256], KC, 256)
                for hf in range(2):
                    j = cbk * 2 + hf
                    pb = psget()
                    for kc in range(KC):
                        mm = P.op("tensor", lambda e, kc=kc, hf=hf, pb=pb, wv=wv: e.matmul(
                            pb.ap[:, 0:TT], lhsT=wv[:, kc, hf * 128:(hf + 1) * 128], rhs=XT3[:, kc, :],
                            start=(kc == 0), stop=(kc == KC - 1)),
                            deps=(wb.ready + pb.free + xt_b.ready) if kc == 0 else (), signal=(kc == KC - 1))
                    ev = P.op("scalar", lambda e, j=j, pb=pb: e.activation(out=UT3[:, j, :], in_=pb.ap[:, 0:TT], func=AF.Gelu_apprx_tanh),
                              deps=[mm] + ut_b.free)
                    pb.free = [ev]
                    u_last = ev
                wb.free = [mm]
            ut_b.free = []
            nvb = D // 512
            st_ev = [None] * 4
            nkh = KC // 2
            for cbk in range(nvb):
                banks = [psget() for _ in range(4)]
                for half in range(2):
                    wb, wv = wload(w_in[:, half * nkh:(half + 1) * nkh, D + cbk * 512:D + (cbk + 1) * 512], nkh, 512)
                    for tt in range(4):
                        pb = banks[tt]
                        for k in range(nkh):
                            kc = half * nkh + k
                            mm = P.op("tensor", lambda e, tt=tt, kc=kc, k=k, pb=pb, wv=wv: e.matmul(
                                pb.ap[:, 0:512], lhsT=XT3[:, kc, tt * 128:(tt + 1) * 128], rhs=wv[:, k, :],
                                start=(kc == 0), stop=(kc == KC - 1)),
                                deps=(wb.ready + (pb.free if half == 0 else []) + xt_b.ready) if k == 0 else (), signal=(k == nkh - 1))
                    wb.free = [mm]
                for tt in range(4):
                    pb = banks[tt]
                    ev = P.op("scalar", lambda e, tt=tt, cbk=cbk, pb=pb: e.activation(
                        out=V3[:, tt, cbk * 512:(cbk + 1) * 512], in_=pb.ap[:, 0:512], func=AF.Gelu_apprx_tanh),
                        deps=[mm] + vb_b.free)
                    pb.free = [ev]
                    st_ev[tt] = P.op("vector", lambda e, tt=tt, cbk=cbk: e.bn_stats(
                        out=lnst[:, tt * 96 + cbk * 6:tt * 96 + (cbk + 1) * 6], in_=V3[:, tt, cbk * 512:(cbk + 1) * 512]), deps=[ev])
            last_mm = mm
            vb_b.free = []
            o2 = c.AH * 128
            WmT = XT[:, 0:o2].rearrange("p (h t) -> p h t", h=c.AH)
            BT = XT[:, o2:o2 + 2 * KC * 128].bitcast(F32).rearrange("p (j t) -> p j t", j=KC)
            p1 = P.dma("sync", lambda e: e.dma_start(out=XT[:, 0:o2], in_=wmt_d[l]), sem_p, deps=[last_mm] + allconst)
            p2 = P.dma("sync", lambda e: e.dma_start(out=XT[:, o2:o2 + 2 * KC * 128].bitcast(F32), in_=bt_d[l]), sem_p,
                       deps=[last_mm] + allconst)
            g_last = None
            sp_last = None
            for tt in range(4):
                sv = lnst[:, tt * 96:tt * 96 + nvb * 6]
                mv = small[:, tt * 4:tt * 4 + 2]
                rs = small[:, tt * 4 + 2:tt * 4 + 3]
                a1 = P.op("vector", lambda e, sv=sv, mv=mv: e.bn_aggr(out=mv, in_=sv), deps=[st_ev[tt]])
                a2a = P.op("scalar", lambda e, mv=mv, rs=rs: e.activation(out=rs, in_=mv[:, 1:2], func=AF.Sqrt, bias=epsc[:, 0:1]), deps=[a1] + allconst)
                a2 = P.op("vector", lambda e, rs=rs: e.reciprocal(out=rs, in_=rs), deps=[a2a])
                a3 = P.op("vector", lambda e, tt=tt, mv=mv, rs=rs: e.tensor_scalar(
                    out=V3[:, tt, :], in0=V3[:, tt, :], scalar1=mv[:, 0:1], scalar2=rs, op0=ALU.subtract, op1=ALU.mult), deps=[a2])
                for jq in range(KC // 4):
                    pb = psget()
                    mm = None
                    for jj in range(4):
                        j = jq * 4 + jj
                        mm = P.op("tensor", lambda e, tt=tt, j=j, jj=jj, pb=pb: e.matmul(
                            pb.ap[:, jj * 128:(jj + 1) * 128], lhsT=V3[:, tt, j * 128:(j + 1) * 128], rhs=WmT[:, j // 2, :],
                            start=True, stop=True), deps=([a3, p1, p2] + pb.free) if jj == 0 else (), signal=(jj == 3))
                    sp_last = mm
                    ev2 = None
                    for jj in range(4):
                        j = jq * 4 + jj
                        tb = sp_tmp.get()
                        ev1 = P.op("vector", lambda e, j=j, jj=jj, pb=pb, tb=tb: e.scalar_tensor_tensor(
                            out=tb.ap, in0=pb.ap[:, jj * 128:(jj + 1) * 128], scalar=lng[:, l * KC + j:l * KC + j + 1],
                            in1=BT[:, j, :], op0=ALU.mult, op1=ALU.add), deps=[mm, p1, p2] + tb.free)
                        ev2 = P.op("vector", lambda e, tt=tt, j=j, tb=tb: e.tensor_tensor(
                            out=UT3[:, j, tt * 128:(tt + 1) * 128], in0=tb.ap, in1=UT3[:, j, tt * 128:(tt + 1) * 128], op=ALU.mult),
                            deps=[ev1, u_last])
                        tb.free = [ev2]
                        g_last = ev2
                    pb.free = [ev2]
            xt_b.free = [sp_last, g_last]
            vb_b.free = [sp_last]
            return [g_last]

        def mixer_b(l):
            w_in = b_w_in[l].rearrange("(k p) n -> p k n", p=128)
            g_last = None
            last_pe = None
            for h in range(c.BH):
                par = h % 2
                base = par * 3600
                xr = [VB[:, base + q * 515:base + (q + 1) * 515] for q in range(2)]
                xc = [VB[:, base + 1030 + q * 512:base + 1030 + (q + 1) * 512] for q in range(2)]
                tb = [VB[:, base + 2054 + q * 512:base + 2054 + (q + 1) * 512] for q in range(2)]
                xcb = VBb[:, 2 * (base + 3078):2 * (base + 3078) + 1024]
                ta = [xr[q][:, 0:512] for q in range(2)]
                wby, wvy = wload(w_in[:, :, h * 256:(h + 1) * 256], KC, 256)
                mm = None
                yev = []
                for hf in range(2):
                    j = h * 2 + hf
                    pb = psget()
                    for kc in range(KC):
                        mm = P.op("tensor", lambda e, kc=kc, hf=hf, pb=pb, wvy=wvy: e.matmul(
                            pb.ap[:, 0:TT], lhsT=wvy[:, kc, hf * 128:(hf + 1) * 128], rhs=XT3[:, kc, :],
                            start=(kc == 0), stop=(kc == KC - 1)),
                            deps=(wby.ready + pb.free + xt_b.ready) if kc == 0 else (), signal=(kc == KC - 1))
                    ev = P.op("scalar", lambda e, j=j, pb=pb: e.activation(out=UT3[:, j, :], in_=pb.ap[:, 0:TT], func=AF.Gelu_apprx_tanh),
                              deps=[mm] + ut_b.free)
                    pb.free = [ev]
                    yev.append(ev)
                wby.free = [mm]
                wbx, wvx = wload(w_in[:, :, D + h * 256:D + (h + 1) * 256], KC, 256)
                xev = []
                for hf in range(2):
                    j = h * 2 + hf
                    bj = l * KC + j
                    pb = psget()
                    for kc in range(KC):
                        mm = P.op("tensor", lambda e, kc=kc, hf=hf, pb=pb, wvx=wvx: e.matmul(
                            pb.ap[:, 0:TT], lhsT=wvx[:, kc, hf * 128:(hf + 1) * 128], rhs=XT3[:, kc, :],
                            start=(kc == 0), stop=(kc == KC - 1)),
                            deps=(wbx.ready + pb.free + xt_b.ready) if kc == 0 else (), signal=(kc == KC - 1))
                    hl = halo[:, bj * 3:bj * 3 + 3]
                    e0 = P.op("scalar", lambda e, hf=hf, hl=hl, xr=xr: e.copy(out=xr[hf][:, 0:3], in_=hl),
                              deps=vbfree[par] + vb_b.free + allconst)
                    e1_ = P.op("scalar", lambda e, hf=hf, pb=pb, xr=xr: e.copy(out=xr[hf][:, 3:515], in_=pb.ap[:, 0:TT]), deps=[mm, e0])
                    e2_ = P.op("scalar", lambda e, hf=hf, hl=hl, xr=xr: e.copy(out=hl, in_=xr[hf][:, 512:515]), deps=[e1_])
                    pb.free = [e1_]

                    def cwl(k, j=j):
                        o = (l * 4 + k) * KC + j
                        return cw[:, o:o + 1]
                    vk = P.op("vector", lambda e, hf=hf, bj=bj, xr=xr, xc=xc, cwl=cwl: e.tensor_scalar(
                        out=xc[hf], in0=xr[hf][:, 0:512], scalar1=cwl(0), scalar2=cb_[:, bj:bj + 1],
                        op0=ALU.mult, op1=ALU.add), deps=[e1_, e2_])
                    for k in range(1, 4):
                        vk = P.op("vector", lambda e, hf=hf, k=k, xr=xr, xc=xc, cwl=cwl: e.scalar_tensor_tensor(
                            out=xc[hf], in0=xr[hf][:, k:k + 512], scalar=cwl(k), in1=xc[hf], op0=ALU.mult, op1=ALU.add), deps=[vk])
                    xev.append(vk)
                wbx.free = [mm]
                cbev = []
                for hf in range(2):
                    cbev.append(P.op("scalar", lambda e, hf=hf, xc=xc, xcb=xcb: e.copy(out=xcb[:, hf * 512:(hf + 1) * 512], in_=xc[hf]),
                                     deps=[xev[hf]]))
                gwb = gw_r.get()
                gwr = gwb.ap[:, 0:512].rearrange("p (k n) -> p k n", k=2)
                gwi = gwb.ap[:, 512:1024].rearrange("p (k n) -> p k n", k=2)
                g1 = P.dma("gpsimd", lambda e, gwr=gwr, h=h: e.dma_start(out=gwr, in_=b_w_r[l, h].rearrange("(k p) n -> p k n", p=128)),
                           gwb.sem, deps=gwb.free)
                g2_ = P.dma("gpsimd", lambda e, gwi=gwi, h=h: e.dma_start(out=gwi, in_=b_w_i[l, h].rearrange("(k p) n -> p k n", p=128)),
                            gwb.sem, deps=gwb.free)
                gmm = None
                for hf in range(2):
                    j = h * 2 + hf
                    bj = l * KC + j
                    pr = psget()
                    pi = psget()
                    for (pp, gw) in ((pr, gwr), (pi, gwi)):
                        for k2 in range(2):
                            gmm = P.op("tensor", lambda e, pp=pp, gw=gw, k2=k2, hf=hf, xcb=xcb: e.matmul(
                                pp.ap[:, 0:TT], lhsT=gw[:, k2, hf * 128:(hf + 1) * 128], rhs=xcb[:, k2 * 512:(k2 + 1) * 512],
                                start=(k2 == 0), stop=(k2 == 1)),
                                deps=([g1, g2_] + cbev + pp.free) if k2 == 0 else (), signal=(k2 == 1))
                    r1 = P.op("scalar", lambda e, hf=hf, pr=pr, bj=bj, ta=ta: e.activation(out=ta[hf], in_=pr.ap[:, 0:TT], func=AF.Sigmoid,
                                                                                       bias=bbr[:, bj:bj + 1]), deps=[gmm] + xev)
                    i1 = P.op("scalar", lambda e, hf=hf, pi=pi, bj=bj, tb=tb: e.activation(out=tb[hf], in_=pi.ap[:, 0:TT], func=AF.Sigmoid,
                                                                                       bias=bbi[:, bj:bj + 1]), deps=[gmm])
                    pr.free = [r1]
                    pi.free = [i1]
                    a1 = P.op("scalar", lambda e, hf=hf, bj=bj, ta=ta: e.activation(out=ta[hf], in_=ta[hf], func=AF.Exp, scale=c8[:, bj:bj + 1]),
                              deps=[r1])
                    b1 = P.op("vector", lambda e, hf=hf, tb=tb, xc=xc: e.tensor_tensor(out=tb[hf], in0=tb[hf], in1=xc[hf], op=ALU.mult),
                              deps=[i1] + cbev)
                    m1 = P.op("vector", lambda e, hf=hf, ta=ta, xc=xc: e.scalar_tensor_tensor(out=xc[hf], in0=ta[hf], scalar=-1.0, in1=ta[hf],
                                                                                          op0=ALU.mult, op1=ALU.mult), deps=[a1, b1])
                    m2 = P.op("vector", lambda e, hf=hf, xc=xc: e.tensor_scalar(out=xc[hf], in0=xc[hf], scalar1=1.0, scalar2=1e-20,
                                                                            op0=ALU.add, op1=ALU.max), deps=[m1])
                    m3 = P.op("scalar", lambda e, hf=hf, xc=xc: e.activation(out=xc[hf], in_=xc[hf], func=AF.Sqrt), deps=[m2])
                    b2 = P.op("vector", lambda e, hf=hf, tb=tb, xc=xc: e.tensor_tensor(out=tb[hf], in0=tb[hf], in1=xc[hf], op=ALU.mult), deps=[m3])
                    hs = hstate[:, bj:bj + 1]
                    s1 = P.op("vector", lambda e, hf=hf, ta=ta, tb=tb, xc=xc, hs=hs: e.tensor_tensor_scan(
                        out=xc[hf], data0=ta[hf], data1=tb[hf], initial=hs, op0=ALU.mult, op1=ALU.add), deps=[b2] + allconst)
                    s2 = P.op("vector", lambda e, hf=hf, xc=xc, hs=hs: e.tensor_copy(out=hs, in_=xc[hf][:, 511:512]), deps=[s1])
                    s3 = P.op("vector", lambda e, hf=hf, j=j, xc=xc: e.tensor_tensor(out=UT3[:, j, :], in0=xc[hf], in1=UT3[:, j, :], op=ALU.mult),
                              deps=[s2, yev[hf]])
                    g_last = s3
                gwb.free = [gmm]
                vbfree[par] = [g_last, gmm]
                last_pe = gmm
            xt_b.free = [last_pe]
            vb_b.free = [g_last, last_pe]
            vbfree[0] = []
            vbfree[1] = []
            return [g_last]

        def moe(L, rd):
            EG, NG = c.EG, c.NG
            lg = VB[:, 3072:3072 + 4 * E].rearrange("p (t e) -> p t e", t=4)
            rb = VB[:, 3072 + 4 * E:3072 + 5 * E]
            exs = VB[:, 3072 + 5 * E:3072 + 9 * E]
            GT = VB[0:E, 3584:3584 + TT]
            bgu = VB[:, 4096:4096 + E * 2 * FC].rearrange("p (e k) -> p e k", e=E)
            bdn = [VB[0:E, q * 512:(q + 1) * 512] for q in range(2)]
            bdn_free = [[], []]
            tmp = [[VB[:, 5120 + (pz * 3 + q) * 512:5120 + (pz * 3 + q + 1) * 512] for q in range(3)] for pz in range(2)]
            tmp_free = [list(vb_b.free), list(vb_b.free)]
            r0 = P.dma("sync", lambda e: e.dma_start(out=rb, in_=router_b[L].partition_broadcast(128)), sem_p3, deps=vb_b.free)
            r1 = P.dma("sync", lambda e: e.dma_start(out=VB[:, 4096:4096 + E * 2 * FC], in_=l_b_gu[L]), sem_p3, deps=vb_b.free)
            gt_ev = []
            lgfree = []
            for tt in range(4):
                lt = lg[:, tt, :]
                m8 = small[:, 16 + tt * 8:16 + tt * 8 + 8]
                q1 = P.op("vector", lambda e, tt=tt, lt=lt: e.tensor_tensor(out=lt, in0=lgbank.ap[:, tt * E:(tt + 1) * E], in1=rb, op=ALU.add),
                          deps=[r0, r1] + rd)
                lgfree.append(q1)
                q2 = P.op("vector", lambda e, lt=lt, m8=m8: e.max(out=m8, in_=lt), deps=[q1])
                nm = small[:, 48 + tt:48 + tt + 1]
                q3 = P.op("vector", lambda e, m8=m8, nm=nm: e.tensor_scalar(out=nm, in0=m8[:, 0:1], scalar1=-1.0, scalar2=None, op0=ALU.mult),
                          deps=[q2])
                ex = exs[:, tt * E:(tt + 1) * E]
                q4 = P.op("scalar", lambda e, lt=lt, ex=ex, nm=nm: e.activation(out=ex, in_=lt, func=AF.Exp, bias=nm), deps=[q3])
                q5 = P.op("vector", lambda e, lt=lt, m8=m8: e.tensor_scalar(out=lt, in0=lt, scalar1=m8[:, c.TOPK - 1:c.TOPK], scalar2=None,
                                                                        op0=ALU.is_ge), deps=[q4])
                sm = small[:, 52 + tt:52 + tt + 1]
                q6 = P.op("vector", lambda e, lt=lt, ex=ex: e.tensor_tensor(out=lt, in0=lt, in1=ex, op=ALU.mult), deps=[q5])
                q7 = P.op("vector", lambda e, lt=lt, sm=sm: e.reduce_sum(out=sm, in_=lt, axis=AX.X), deps=[q6])
                q8 = P.op("vector", lambda e, sm=sm: e.reciprocal(out=sm, in_=sm), deps=[q7])
                q9 = P.op("vector", lambda e, lt=lt, sm=sm: e.tensor_scalar(out=lt, in0=lt, scalar1=sm, scalar2=None, op0=ALU.mult), deps=[q8])
                pb = psget()
                t1 = P.op("tensor", lambda e, lt=lt, pb=pb: e.transpose(out=pb.ap[0:E, 0:128], in_=lt, identity=ident[:]), deps=[q9] + pb.free)
                t2 = P.op("scalar", lambda e, tt=tt, pb=pb: e.copy(out=GT[:, tt * 128:(tt + 1) * 128], in_=pb.ap[0:E, 0:128]), deps=[t1])
                pb.free = [t2]
                gt_ev.append(t2)
                s0 = P.op("scalar", lambda e, tt=tt: e.mul(out=ACC[:, tt, :], in_=ACC[:, tt, :], mul=c.ALPHA), deps=rd + acc_b[tt].ready)
                acc_b[tt].ready = [s0]
            lgbank.free = lgfree
            H3 = UT[:, 0:EG * FC * TT].rearrange("p (k t) -> p k t", k=EG * FC)
            hdn_free = list(ut_b.free)
            fin = None
            pz = 0
            dmm = None
            wd = ex_w_down[L].rearrange("e (k p) n -> p (e k) n", p=128)
            nk = EG * FC
            for g in range(NG):
                h_last = None
                for ei in range(EG):
                    e_ = g * EG + ei
                    gb = psget()
                    sel = ident[0:E, e_:e_ + 1].to_broadcast([E, 128])
                    gm = P.op("tensor", lambda e, gb=gb, sel=sel: e.matmul(gb.ap[:, 0:TT], lhsT=sel, rhs=GT, start=True, stop=True),
                              deps=gt_ev + gb.free + allconst)
                    wsrc = ex_w_gu[L][e_].rearrange("(k p) n -> p k n", p=128)
                    banks = {}
                    for cbk in range(FC):
                        wb, wv = wload(wsrc[:, :, cbk * 256:(cbk + 1) * 256], KC, 256)
                        mm = None
                        for hf in range(2):
                            ch = cbk * 2 + hf
                            pb = psget()
                            for kc in range(KC):
                                mm = P.op("tensor", lambda e, kc=kc, hf=hf, pb=pb, wv=wv: e.matmul(
                                    pb.ap[:, 0:TT], lhsT=wv[:, kc, hf * 128:(hf + 1) * 128], rhs=XT3[:, kc, :],
                                    start=(kc == 0), stop=(kc == KC - 1)),
                                    deps=(wb.ready + pb.free + xt_b.ready) if kc == 0 else (), signal=(kc == KC - 1))
                            banks[ch] = (pb, mm)
                        wb.free = [mm]
                    for cc in range(FC):
                        pg_, mg_ = banks[cc]
                        pu_, mu_ = banks[FC + cc]
                        t0, t1_, t2_ = tmp[pz]
                        w0 = P.op("vector", lambda e, pg_=pg_, t0=t0, e_=e_, cc=cc: e.tensor_scalar(
                            out=t0, in0=pg_.ap[:, 0:TT], scalar1=bgu[:, e_, cc:cc + 1], scalar2=7.0, op0=ALU.add, op1=ALU.min),
                            deps=[mg_, r0, r1] + tmp_free[pz])
                        pg_.free = [w0]
                        w1 = P.op("scalar", lambda e, t0=t0, t1_=t1_: e.activation(out=t1_, in_=t0, func=AF.Sigmoid, scale=1.702), deps=[w0])
                        w2 = P.op("vector", lambda e, pu_=pu_, t2_=t2_, e_=e_, cc=cc: e.tensor_scalar(
                            out=t2_, in0=pu_.ap[:, 0:TT], scalar1=bgu[:, e_, FC + cc:FC + cc + 1], scalar2=7.0, op0=ALU.add, op1=ALU.min),
                            deps=[mu_, w0])
                        pu_.free = [w2]
                        w3 = P.op("vector", lambda e, t2_=t2_: e.tensor_scalar(out=t2_, in0=t2_, scalar1=-7.0, scalar2=1.0, op0=ALU.max, op1=ALU.add),
                                  deps=[w2])
                        w4 = P.op("vector", lambda e, t0=t0, t1_=t1_: e.tensor_tensor(out=t0, in0=t0, in1=t1_, op=ALU.mult), deps=[w1, w3])
                        w5 = P.op("vector", lambda e, t0=t0, t2_=t2_: e.tensor_tensor(out=t0, in0=t0, in1=t2_, op=ALU.mult), deps=[w4])
                        w6 = P.op("vector", lambda e, t0=t0, gb=gb, ei=ei, cc=cc: e.tensor_tensor(
                            out=H3[:, ei * FC + cc, :], in0=t0, in1=gb.ap[:, 0:TT], op=ALU.mult), deps=[w5, gm] + hdn_free)
                        tmp_free[pz] = [w6]
                        pz ^= 1
                        h_last = w6
                    gb.free = [h_last]
                hdn_free = []
                nkh2 = nk // 2
                for cbk in range(D // 512):
                    bq = cbk % 2
                    bl = None
                    if g == 0:
                        bl = P.dma("sync", lambda e, bq=bq, cbk=cbk: e.dma_start(out=bdn[bq], in_=ex_b_down[L][:, cbk * 512:(cbk + 1) * 512]),
                                   bsem[bq], deps=bdn_free[bq] + [r0, r1] + rd)
                    banks = [psget() for _ in range(4)]
                    for half in range(2):
                        wb, wv = wload(wd[:, g * nk + half * nkh2:g * nk + (half + 1) * nkh2, cbk * 512:(cbk + 1) * 512], nkh2, 512)
                        for tt in range(4):
                            pb = banks[tt]
                            if g == 0 and half == 0:
                                P.op("tensor", lambda e, tt=tt, pb=pb, bq=bq: e.matmul(pb.ap[:, 0:512], lhsT=GT[:, tt * 128:(tt + 1) * 128], rhs=bdn[bq],
                                                                                       start=True, stop=False),
                                     deps=[bl] + gt_ev + pb.free, signal=False)
                            for k in range(nkh2):
                                kk = half * nkh2 + k
                                dmm = P.op("tensor", lambda e, tt=tt, k=k, kk=kk, pb=pb, wv=wv, g=g: e.matmul(
                                    pb.ap[:, 0:512], lhsT=H3[:, kk, tt * 128:(tt + 1) * 128], rhs=wv[:, k, :],
                                    start=(kk == 0 and g != 0), stop=(kk == nk - 1)),
                                    deps=(wb.ready + [h_last] + (pb.free if (g != 0 and half == 0) else [])) if k == 0 else (),
                                    signal=(k == nkh2 - 1))
                        wb.free = [dmm]
                    for tt in range(4):
                        pb = banks[tt]
                        ev = P.op("vector", lambda e, tt=tt, cbk=cbk, pb=pb: e.tensor_tensor(
                            out=ACC[:, tt, cbk * 512:(cbk + 1) * 512], in0=ACC[:, tt, cbk * 512:(cbk + 1) * 512], in1=pb.ap[:, 0:512], op=ALU.add),
                            deps=[dmm] + acc_b[tt].ready)
                        pb.free = [ev]
                        fin = ev
                    if g == 0:
                        bdn_free[bq] = [dmm]
                hdn_free = [dmm]
            ut_b.free = [dmm]
            xt_b.free = [dmm]
            for tt in range(4):
                acc_b[tt].ready = [fin]
                acc_b[tt].free = []
            vb_b.free = [dmm, fin]

        out_ev = []
        for s in range(NST):
            for tt in range(4):
                r0_ = s * TT + tt * 128
                ev = P.dma("sync", lambda e, tt=tt, r0_=r0_: e.dma_start(out=ACC[:, tt, :], in_=x[r0_:r0_ + 128, :]), sem_in[tt],
                           deps=acc_b[tt].free + acc_b[tt].ready)
                acc_b[tt].ready = [ev]
                acc_b[tt].free = []
            for L in layers:
                l = L // 2
                if upto < 1:
                    break
                build_xt()
                if upto < 2:
                    break
                if L % 2 == 0:
                    ut_ready = mixer_a(l)
                else:
                    ut_ready = mixer_b(l)
                if upto < 3:
                    break
                wout_phase(a_w_out[l] if L % 2 == 0 else b_w_out[l], ut_ready)
                if upto < 4:
                    break
                layer_norm(ln1_g[L], ln1_b[L])
                if upto < 5:
                    break
                rd = build_xt(router_layer=L)
                if upto < 6:
                    break
                moe(L, rd)
                if upto < 7:
                    break
                layer_norm(ln2_g[L], ln2_b[L])
            for tt in range(4):
                r0_ = s * TT + tt * 128
                ev = P.dma("sync", lambda e, tt=tt, r0_=r0_: e.dma_start(out=out[r0_:r0_ + 128, :], in_=ACC[:, tt, :]), sem_out[tt],
                           deps=acc_b[tt].ready)
                acc_b[tt].free = [ev]
                out_ev.append(ev)
        P.wait_only("sync", out_ev)
        with nc.Block() as block:
            P.replay(block)
    return nc, P


def _pj(v, KC):
    v = np.asarray(v, np.float32)
    lead = int(np.prod(v.shape[:-1])) if v.ndim > 1 else 1
    v = v.reshape(lead, KC, 128)
    return np.ascontiguousarray(v.transpose(2, 0, 1).reshape(128, lead * KC))


def prep_inputs(cfg, inputs):
    c = cfg
    KC, E, FC = c.KC, c.E, c.FC
    f = lambda k: np.ascontiguousarray(inputs[k], dtype=np.float32)
    m = {}
    for k in ["a_w_in", "a_w_out", "b_w_in", "b_w_r", "b_w_i", "b_w_out", "ln1_g", "ln1_b", "ln2_g", "ln2_b",
              "router_b", "ex_w_gu", "ex_w_down", "ex_b_down", "a_b_s"]:
        m[k] = f(k)
    m["l_a_ln_g"] = _pj(f("a_ln_g"), KC)
    m["l_a_ln_b"] = _pj(f("a_ln_b"), KC)
    m["l_a_w_sT"] = np.ascontiguousarray(f("a_w_s").transpose(0, 3, 1, 2).reshape(c.nA, 128, c.AH * 128))
    m["l_conv_w"] = _pj(f("b_conv_w"), KC)
    m["l_conv_b"] = _pj(f("b_conv_b"), KC)
    m["l_b_r"] = _pj(f("b_b_r").reshape(c.nB, -1), KC)
    m["l_b_i"] = _pj(f("b_b_i").reshape(c.nB, -1), KC)
    m["l_lam"] = _pj(f("b_lambda"), KC)
    rw = f("router_w").reshape(c.DEPTH, KC, 128, E)
    m["l_router_w"] = np.ascontiguousarray(rw.transpose(0, 2, 1, 3).reshape(c.DEPTH, 128, KC * E))
    bg = f("ex_b_gu").reshape(c.DEPTH, E, 2 * FC, 128)
    m["l_b_gu"] = np.ascontiguousarray(bg.transpose(0, 3, 1, 2).reshape(c.DEPTH, 128, E * 2 * FC))
    m["c_ident"] = np.eye(128, dtype=np.float32)
    m["c_triu"] = np.triu(np.ones((128, 128), np.float32))
    return m


def run(cfg, inputs, layers=None, trace=False, upto=99):
    nc, P = build_program(cfg, layers, upto)
    x = np.ascontiguousarray(inputs["x"], dtype=np.float32)
    xs = x.reshape(-1, cfg.D)
    n = cfg.n_cores
    per = xs.shape[0] // n
    assert per == cfg.NTOK
    base = prep_inputs(cfg, inputs)
    in_maps = []
    for ci in range(n):
        m = dict(base)
        m["x"] = xs[ci * per:(ci + 1) * per]
        in_maps.append(m)
    res = run_bass_kernel_spmd(nc, in_maps, core_ids=list(range(n)), trace=trace)
    y = np.concatenate([r["out"] for r in res.results], axis=0).reshape(x.shape)
    return y, res


def kernel(**inputs):
    cfg = Cfg()
    y, _ = run(cfg, inputs)
    return y.astype(np.float32)
```

```python
import math
import numpy as np
from contextlib import ExitStack
import concourse.bass as bass
import concourse.mybir as mybir
from concourse.bass_utils import run_bass_kernel_spmd

F32 = mybir.dt.float32
BF16 = mybir.dt.bfloat16
AF = mybir.ActivationFunctionType
ALU = mybir.AluOpType
AX = mybir.AxisListType

ENGS = ["sync", "scalar", "vector", "gpsimd", "tensor"]
SEM_LIMIT = 30000


class Cfg:
    def __init__(self, D=4096, NTOK=8192, DEPTH=4, AH=16, BH=16, E=32, F=384, TOPK=4, n_cores=2):
        self.D = D
        self.KC = D // 128
        self.NTOK = NTOK
        self.TT = 512
        self.NST = NTOK // 512
        self.DEPTH = DEPTH
        self.AH = AH
        self.BH = BH
        self.E = E
        self.F = F
        self.FC = F // 128
        self.TOPK = TOPK
        self.n_cores = n_cores
        self.nA = (DEPTH + 1) // 2
        self.nB = DEPTH // 2
        self.ALPHA = float((2 * DEPTH) ** 0.25)
        self.EPS = 1e-5
        self.EG = min(8, E)
        self.NG = E // self.EG


class Ev:
    __slots__ = ("sem", "val")

    def __init__(self, sem, val):
        self.sem = sem
        self.val = val


def _flat(deps):
    out = []
    if deps is None:
        return out
    if isinstance(deps, Ev):
        return [deps]
    for d in deps:
        if d is None:
            continue
        if isinstance(d, Ev):
            out.append(d)
        else:
            out.extend(_flat(d))
    return out


class Prog:
    def __init__(self, nc, stack):
        self.nc = nc
        self.stack = stack
        self.q = {e: [] for e in ENGS}
        self.cur = {}
        self.waited = {e: {} for e in ENGS}
        self.nsem = 0
        self.ninst = 0

    def new_sem(self, name):
        self.nsem += 1
        return self.stack.enter_context(self.nc.semaphore(f"{name}_{self.nsem}"))

    def _eng_sem(self, eng):
        c = self.cur.get(eng)
        if c is None or c[1] >= SEM_LIMIT:
            c = [self.new_sem("e" + eng), 0]
            self.cur[eng] = c
        return c

    def _waits(self, eng, deps):
        wd = self.waited[eng]
        best = {}
        for d in _flat(deps):
            k = id(d.sem)
            if wd.get(k, 0) >= d.val:
                continue
            if k not in best or best[k][1] < d.val:
                best[k] = (d.sem, d.val)
        w = []
        for k, (sem, val) in best.items():
            wd[k] = val
            w.append((sem, val))
        return w

    def op(self, eng, fn, deps=(), signal=True):
        waits = self._waits(eng, deps)
        ev = None
        inc = None
        if signal:
            c = self._eng_sem(eng)
            c[1] += 1
            ev = Ev(c[0], c[1])
            inc = (c[0], 1)
        self.q[eng].append((fn, waits, inc))
        self.ninst += 1
        return ev

    def dma(self, eng, fn, sem_state, deps=()):
        waits = self._waits(eng, deps)
        sem_state[1] += 16
        ev = Ev(sem_state[0], sem_state[1])
        self.q[eng].append((fn, waits, (sem_state[0], 16)))
        self.ninst += 1
        return ev

    def dsem(self, name="d"):
        return [self.new_sem(name), 0]

    def wait_only(self, eng, deps):
        waits = self._waits(eng, deps)
        if waits:
            self.q[eng].append((None, waits, None))

    def replay(self, block):
        for eng in ENGS:
            items = self.q[eng]
            if not items:
                continue

            def body(e, items=items):
                for fn, waits, inc in items:
                    for (s, v) in waits:
                        e.wait_ge(s, v)
                    if fn is None:
                        continue
                    ins = fn(e)
                    if inc is not None:
                        ins.then_inc(inc[0], inc[1])

            getattr(block, eng)(body)


class Buf:
    def __init__(self, ap):
        self.ap = ap
        self.ready = []
        self.free = []


class Ring:
    def __init__(self, aps):
        self.bufs = [Buf(a) for a in aps]
        self.i = 0

    def get(self):
        b = self.bufs[self.i % len(self.bufs)]
        self.i += 1
        return b


def build_program(cfg, layers=None, upto=99):
    c = cfg
    D, KC, TT, NST, E, FC = c.D, c.KC, c.TT, c.NST, c.E, c.FC
    layers = list(range(c.DEPTH)) if layers is None else layers
    nc = bass.Bass("TRN2", target_bir_lowering=False)

    def din(name, shape):
        return nc.dram_tensor(name, list(shape), F32, kind="ExternalInput").ap()

    nA1, nB1 = max(c.nA, 1), max(c.nB, 1)
    x = din("x", [c.NTOK, D])
    a_w_in = din("a_w_in", [c.nA, D, 2 * D])
    a_w_out = din("a_w_out", [c.nA, D, D])
    b_w_in = din("b_w_in", [c.nB, D, 2 * D])
    b_w_r = din("b_w_r", [c.nB, c.BH, 256, 256])
    b_w_i = din("b_w_i", [c.nB, c.BH, 256, 256])
    b_w_out = din("b_w_out", [c.nB, D, D])
    ln1_g = din("ln1_g", [c.DEPTH, D])
    ln1_b = din("ln1_b", [c.DEPTH, D])
    ln2_g = din("ln2_g", [c.DEPTH, D])
    ln2_b = din("ln2_b", [c.DEPTH, D])
    router_b = din("router_b", [c.DEPTH, E])
    ex_w_gu = din("ex_w_gu", [c.DEPTH, E, D, 2 * c.F])
    ex_w_down = din("ex_w_down", [c.DEPTH, E, c.F, D])
    ex_b_down = din("ex_b_down", [c.DEPTH, E, D])
    a_b_s = din("a_b_s", [c.nA, c.AH, 128])
    l_a_ln_g = din("l_a_ln_g", [128, nA1 * KC])
    l_a_ln_b = din("l_a_ln_b", [128, nA1 * KC])
    l_a_w_sT = din("l_a_w_sT", [nA1, 128, c.AH * 128])
    l_conv_w = din("l_conv_w", [128, nB1 * 4 * KC])
    l_conv_b = din("l_conv_b", [128, nB1 * KC])
    l_b_r = din("l_b_r", [128, nB1 * KC])
    l_b_i = din("l_b_i", [128, nB1 * KC])
    l_lam = din("l_lam", [128, nB1 * KC])
    l_router_w = din("l_router_w", [c.DEPTH, 128, KC * E])
    l_b_gu = din("l_b_gu", [c.DEPTH, 128, E * 2 * FC])
    c_ident = din("c_ident", [128, 128])
    c_triu = din("c_triu", [128, 128])
    out = nc.dram_tensor("out", [c.NTOK, D], F32, kind="ExternalOutput").ap()
    big = {"a_w_in": a_w_in, "a_w_out": a_w_out, "b_w_in": b_w_in, "b_w_out": b_w_out,
           "ex_w_gu": ex_w_gu, "ex_w_down": ex_w_down}
    bigb = {}
    for nm_, ap_ in big.items():
        bigb[nm_] = [nc.dram_tensor(f"{nm_}_bf{i_}", list(ap_.shape[1:]), BF16).ap() for i_ in range(ap_.shape[0])]
    wmt_d = nc.dram_tensor("wmt_d", [nA1, 128, c.AH * 128], BF16).ap()
    bt_d = nc.dram_tensor("bt_d", [nA1, 128, KC * 128], F32).ap()

    st = ExitStack()
    with st:
        P = Prog(nc, st)

        def sb(name, shape, dt):
            return st.enter_context(nc.sbuf_tensor(name, list(shape), dt))

        ACC = sb("ACC", [128, 4, D], F32)
        XT = sb("XT", [128, KC * TT], BF16)
        UT = sb("UT", [128, max(KC * TT, c.EG * FC * TT)], BF16)
        VB = sb("VB", [128, 8192], F32)
        WR = [sb(f"WR{i}", [128, max(KC, c.EG * FC) * 256], BF16) for i in range(2)]
        ident = sb("ident", [128, 128], F32)
        XT3 = XT[:].rearrange("p (k t) -> p k t", k=KC)
        UT3 = UT[:, 0:KC * TT].rearrange("p (k t) -> p k t", k=KC)
        VBb = VB[:].bitcast(BF16)

        lng = sb("lng", [128, nA1 * KC], F32)
        lnb = sb("lnb", [128, nA1 * KC], F32)
        cw = sb("cw", [128, nB1 * 4 * KC], F32)
        cb_ = sb("cb", [128, nB1 * KC], F32)
        bbr = sb("bbr", [128, nB1 * KC], F32)
        bbi = sb("bbi", [128, nB1 * KC], F32)
        c8 = sb("c8", [128, nB1 * KC], F32)
        hstate = sb("hstate", [128, nB1 * KC], F32)
        halo = sb("halo", [128, nB1 * KC * 3], F32)
        small = sb("small", [128, 64], F32)
        epsc = sb("epsc", [128, 1], F32)
        lnst = sb("lnst", [128, 4 * 96], F32)
        sp_tmp_t = sb("sp_tmp", [128, 2 * 128], F32)
        gw_t0 = sb("gwt", [128, 2 * 1024], BF16)

        wring = Ring([w[:] for w in WR])
        wsem = [P.dsem("w0"), P.dsem("w1")]
        PSB = [st.enter_context(nc.psum_tensor(f"ps{i}", [128, 512], F32)) for i in range(8)]
        psring = Ring([p[:] for p in PSB[0:7]])
        lgbank = Buf(PSB[7][:])

        acc_b = [Buf(ACC[:, tt, :]) for tt in range(4)]
        xt_b = Buf(XT3)
        ut_b = Buf(UT3)
        vb_b = Buf(VB[:])
        sem_in = [P.dsem(f"in{i}") for i in range(4)]
        sem_out = [P.dsem(f"out{i}") for i in range(4)]
        sem_c = P.dsem("const")
        sem_pr = P.dsem("prep")
        sem_p = P.dsem("par")
        sem_p2 = P.dsem("par2")
        sem_p3 = P.dsem("par3")
        bsem = [P.dsem("bd0"), P.dsem("bd1")]
        sp_tmp = Ring([sp_tmp_t[:, 0:128], sp_tmp_t[:, 128:256]])
        gw_r = Ring([gw_t0[:, 0:1024], gw_t0[:, 1024:2048]])
        for i_, b_ in enumerate(gw_r.bufs):
            b_.sem = P.dsem(f"gw{i_}")
        vbfree = [[], []]

        def wload(src3, nk, ncols):
            slot = wring.i % 2
            b = wring.get()
            view = b.ap[:, 0:nk * ncols].rearrange("p (k n) -> p k n", k=nk)
            Lc = cur_layer[0]
            emit_conv(Lc, 10 ** 9)
            ev = P.dma("gpsimd", lambda e, view=view, src3=src3: e.dma_start(out=view, in_=src3), wsem[slot],
                       deps=b.free + [cvt_done[Lc]])
            nxt = layers[(layers.index(Lc) + 1) % len(layers)]
            emit_conv(nxt, 1)
            b.free = []
            b.ready = [ev]
            return b, view

        def psget():
            return psring.get()

        CH = 8192
        conv_list = {L_: [] for L_ in range(c.DEPTH)}

        def _add_conv(L_, src_, dst_):
            tot = int(np.prod(src_.shape))
            names = " ".join(f"d{i}" for i in range(len(src_.shape)))
            sf = src_.rearrange(f"{names} -> ({names})").rearrange("(p n) -> p n", p=128)
            df = dst_.rearrange(f"{names} -> ({names})").rearrange("(p n) -> p n", p=128)
            per = tot // 128
            for o in range(0, per, CH):
                w_ = min(CH, per - o)
                conv_list[L_].append(lambda e, sf=sf, df=df, o=o, w_=w_: e.dma_start(out=df[:, o:o + w_], in_=sf[:, o:o + w_]))

        for L_ in range(c.DEPTH):
            l_ = L_ // 2
            if L_ % 2 == 0:
                _add_conv(L_, big["a_w_in"][l_], bigb["a_w_in"][l_])
                _add_conv(L_, big["a_w_out"][l_], bigb["a_w_out"][l_])
            else:
                _add_conv(L_, big["b_w_in"][l_], bigb["b_w_in"][l_])
                _add_conv(L_, big["b_w_out"][l_], bigb["b_w_out"][l_])
            _add_conv(L_, big["ex_w_gu"][L_], bigb["ex_w_gu"][L_])
            _add_conv(L_, big["ex_w_down"][L_], bigb["ex_w_down"][L_])
        sem_cvL = [P.dsem(f"cvt{L_}") for L_ in range(c.DEPTH)]
        cvt_done = [Ev(sem_cvL[L_][0], 16 * len(conv_list[L_])) for L_ in range(c.DEPTH)]
        conv_pos = [0] * c.DEPTH
        cur_layer = [0]

        def emit_conv(L_, n):
            while n > 0 and conv_pos[L_] < len(conv_list[L_]):
                P.dma("gpsimd", conv_list[L_][conv_pos[L_]], sem_cvL[L_])
                conv_pos[L_] += 1
                n -= 1

        emit_conv(layers[0], 10 ** 9)
        a_w_in, a_w_out, b_w_in, b_w_out, ex_w_gu, ex_w_down = (bigb[k_] for k_ in
                                                                  ["a_w_in", "a_w_out", "b_w_in", "b_w_out", "ex_w_gu", "ex_w_down"])

        cev = [P.dma("sync", lambda e: e.dma_start(out=ident[:], in_=c_ident[:, :]), sem_c)]

        def cload(dst, src):
            cev.append(P.dma("sync", lambda e: e.dma_start(out=dst, in_=src), sem_c))

        cload(lng[:], l_a_ln_g[:, :])
        cload(lnb[:], l_a_ln_b[:, :])
        cload(cw[:], l_conv_w[:, :])
        cload(cb_[:], l_conv_b[:, :])
        cload(bbr[:], l_b_r[:, :])
        cload(bbi[:], l_b_i[:, :])
        cload(c8[:], l_lam[:, :])
        const_ev = list(cev)
        e1 = P.op("scalar", lambda e: e.activation(out=c8[:], in_=c8[:], func=AF.Exp, scale=-1.0), deps=cev)
        e2 = P.op("scalar", lambda e: e.activation(out=c8[:], in_=c8[:], func=AF.Ln, bias=1.0), deps=[e1])
        e3 = P.op("scalar", lambda e: e.mul(out=c8[:], in_=c8[:], mul=-8.0), deps=[e2])
        e4 = P.op("vector", lambda e: e.memset(hstate[:], 0.0))
        e5 = P.op("vector", lambda e: e.memset(halo[:], 0.0), deps=[e4])
        e6 = P.op("vector", lambda e: e.memset(epsc[:], c.EPS), deps=[e5])
        const_ev += [e3, e4, e5, e6]

        prep_ev = []
        AHn = c.AH
        for l in range(c.nA):
            WT32 = ACC[:, 0, 0:AHn * 128].rearrange("p (h t) -> p h t", h=AHn)
            WmTb = VBb[:, 0:AHn * 128].rearrange("p (h t) -> p h t", h=AHn)
            bsbc = ACC[:, 1, 0:AHn * 128].rearrange("p (h t) -> p h t", h=AHn)
            BTt = ACC[:, 2, 0:KC * 128].rearrange("p (j t) -> p j t", j=KC)
            triu = VB[:, 4096:4096 + 128]
            ones = VB[:, 4096 + 128:4096 + 256]
            d0 = list(prep_ev)
            l1 = P.dma("sync", lambda e, l=l: e.dma_start(out=ACC[:, 0, 0:AHn * 128], in_=l_a_w_sT[l]), sem_pr, deps=d0)
            l2 = P.dma("sync", lambda e, triu=triu: e.dma_start(out=triu, in_=c_triu[:, :]), sem_pr, deps=d0)
            l3 = P.dma("sync", lambda e, l=l: e.dma_start(out=ACC[:, 1, 0:AHn * 128],
                                                          in_=a_b_s[l].rearrange("h t -> (h t)").partition_broadcast(128)),
                       sem_pr, deps=d0)
            m0 = P.op("vector", lambda e, ones=ones: e.memset(ones, 1.0), deps=d0 + const_ev)
            mk = m0
            for h in range(AHn):
                mk = P.op("vector", lambda e, h=h, WT32=WT32, triu=triu: e.tensor_tensor(out=WT32[:, h, :], in0=WT32[:, h, :], in1=triu, op=ALU.mult),
                          deps=[l1, l2, l3, mk])
            cvt = P.op("vector", lambda e, WT32=WT32, WmTb=WmTb: e.tensor_copy(out=WmTb, in_=WT32), deps=[mk])
            bev = []
            for q in range((AHn * 128 + 511) // 512):
                pb = psget()
                ncol = min(512, AHn * 128 - q * 512)
                mm = P.op("tensor", lambda e, q=q, pb=pb, ncol=ncol, ones=ones: e.matmul(
                    pb.ap[:, 0:ncol], lhsT=ones, rhs=ACC[:, 0, q * 512:q * 512 + ncol], start=True, stop=True),
                    deps=[mk, m0] + pb.free)
                rl = []
                for hh in range(ncol // 128):
                    h = q * 4 + hh
                    for jj in range(2):
                        j = h * 2 + jj
                        rl.append(P.op("vector", lambda e, pb=pb, hh=hh, j=j, h=h, l=l, BTt=BTt, bsbc=bsbc: e.scalar_tensor_tensor(
                            out=BTt[:, j, :], in0=pb.ap[:, hh * 128:(hh + 1) * 128], scalar=lnb[:, l * KC + j:l * KC + j + 1],
                            in1=bsbc[:, h, :], op0=ALU.mult, op1=ALU.add), deps=[mm, l3] + const_ev))
                pb.free = rl
                bev += rl
            s1 = P.dma("sync", lambda e, l=l: e.dma_start(out=wmt_d[l], in_=VBb[:, 0:AHn * 128]), sem_pr, deps=[cvt])
            s2 = P.dma("sync", lambda e, l=l: e.dma_start(out=bt_d[l], in_=ACC[:, 2, 0:KC * 128]), sem_pr, deps=bev)
            prep_ev = [s1, s2]
        for b in acc_b:
            b.free = list(prep_ev)
        vb_b.free = list(prep_ev)
        allconst = const_ev + prep_ev

        def build_xt(router_layer=None):
            evs_all = []
            rd = []
            if router_layer is not None:
                L = router_layer
                rw = VB[:, 0:KC * E].rearrange("p (k e) -> p k e", k=KC)
                rwev = P.dma("sync", lambda e: e.dma_start(out=VB[:, 0:KC * E], in_=l_router_w[L]), sem_p, deps=vb_b.free)
                stg_r = Ring([VB[:, 2048:2560], VB[:, 2560:3072]])
                for sgb in stg_r.bufs:
                    sgb.free = list(vb_b.free)
            for tt in range(4):
                for q in range(KC // 4):
                    pb = psget()
                    tl = None
                    for i in range(4):
                        kc = q * 4 + i
                        tl = P.op("tensor", lambda e, tt=tt, kc=kc, i=i, pb=pb: e.transpose(
                            out=pb.ap[:, i * 128:(i + 1) * 128], in_=ACC[:, tt, kc * 128:(kc + 1) * 128], identity=ident[:]),
                            deps=(acc_b[tt].ready + pb.free + allconst) if i == 0 else (), signal=(i == 3))
                    ce = P.op("scalar", lambda e, tt=tt, q=q, pb=pb: e.copy(
                        out=XT3[:, q * 4:(q + 1) * 4, tt * 128:(tt + 1) * 128],
                        in_=pb.ap.rearrange("p (k t) -> p k t", k=4)), deps=[tl] + xt_b.free)
                    fr = [ce]
                    if router_layer is not None:
                        sg = stg_r.get()
                        c2 = P.op("vector", lambda e, pb=pb, sg=sg: e.tensor_copy(out=sg.ap, in_=pb.ap), deps=[tl, ce] + sg.free)
                        fr.append(c2)
                        ml = None
                        for i in range(4):
                            kc = q * 4 + i
                            ml = P.op("tensor", lambda e, i=i, kc=kc, sg=sg, tt=tt, rw=rw: e.matmul(
                                lgbank.ap[:, tt * E:(tt + 1) * E], lhsT=sg.ap[:, i * 128:(i + 1) * 128], rhs=rw[:, kc, :],
                                start=(kc == 0), stop=(kc == KC - 1)),
                                deps=([c2, rwev] + (lgbank.free if (kc == 0 and tt == 0) else [])) if i == 0 else (), signal=(i == 3))
                        sg.free = [ml]
                        rd.append(ml)
                    pb.free = fr
                    evs_all.append(ce)
                    rd.append(tl)
            xt_b.free = []
            xt_b.ready = evs_all[-1:]
            for tt in range(4):
                acc_b[tt].free = acc_b[tt].free + rd[-2:]
            return rd[-2:]

        def layer_norm(gsrc, bsrc):
            g2 = VB[0:KC, 7168:7168 + 128]
            b2 = VB[0:KC, 7168 + 128:7168 + 256]
            import os as _os
            _dbg = int(_os.environ.get("LNDBG", "9"))
            pg = pb_ = None
            if _dbg != 0:
                pg = P.dma("sync", lambda e: e.dma_start(out=g2, in_=gsrc.rearrange("(k c) -> k c", c=128)), sem_p2, deps=vb_b.free)
                pb_ = P.dma("sync", lambda e: e.dma_start(out=b2, in_=bsrc.rearrange("(k c) -> k c", c=128)), sem_p2, deps=vb_b.free)
            nblk = D // 512
            norm_ev = []
            for tt in range(4):
                sv = lnst[:, tt * 96:(tt + 1) * 96]
                se = None
                _lnv = int(_os.environ.get("LNV", "0"))
                for blk in range(nblk):
                    if _lnv == 2:
                        se = P.op("vector", lambda e, tt=tt, blk=blk, sv=sv: e.tensor_copy(
                            out=sv[:, blk * 6:(blk + 1) * 6], in_=ACC[:, tt, blk * 512:blk * 512 + 6]),
                            deps=acc_b[tt].ready if blk == 0 else (), signal=(blk == nblk - 1))
                        continue
                    se = P.op("vector", lambda e, tt=tt, blk=blk, sv=sv: e.bn_stats(
                        out=sv[:, blk * 6:(blk + 1) * 6], in_=ACC[:, tt, blk * 512:(blk + 1) * 512]),
                        deps=(acc_b[tt].ready if _lnv != 1 else ()) if blk == 0 else (), signal=(blk == nblk - 1))
                mv = small[:, tt * 4:tt * 4 + 2]
                rs = small[:, tt * 4 + 2:tt * 4 + 3]
                _sub = int(_os.environ.get("LNSUB", "9"))
                if _sub < 2:
                    norm_ev.append(se); continue
                a1 = P.op("vector", lambda e, sv=sv, mv=mv: e.bn_aggr(out=mv, in_=sv[:, 0:nblk * 6]), deps=[se])
                if _sub < 3:
                    norm_ev.append(a1); continue
                a2a = P.op("scalar", lambda e, mv=mv, rs=rs: e.activation(out=rs, in_=mv[:, 1:2], func=AF.Sqrt, bias=epsc[:, 0:1]), deps=[a1] + allconst)
                if _sub < 4:
                    norm_ev.append(a2a); continue
                a2 = P.op("vector", lambda e, rs=rs: e.reciprocal(out=rs, in_=rs), deps=[a2a])
                if _sub < 5:
                    norm_ev.append(a2); continue
                a3 = P.op("vector", lambda e, tt=tt, mv=mv, rs=rs: e.tensor_scalar(
                    out=ACC[:, tt, :], in0=ACC[:, tt, :], scalar1=mv[:, 0:1], scalar2=rs, op0=ALU.subtract, op1=ALU.mult),
                    deps=[a2] + acc_b[tt].free)
                norm_ev.append(a3)
            fin = None
            mg = mb = None
            import os as _os
            _dbg = int(_os.environ.get("LNDBG", "9"))
            if _dbg < 2:
                for tt in range(4):
                    acc_b[tt].ready = [norm_ev[-1]]
                    acc_b[tt].free = []
                vb_b.free = [pg, pb_]
                return
            for blk in range(nblk):
                bg = psget()
                bb = psget()
                for qd in range(4):
                    kc = blk * 4 + qd
                    sel = ident[0:KC, kc:kc + 1].to_broadcast([KC, 128])
                    mg = P.op("tensor", lambda e, qd=qd, sel=sel, bg=bg: e.matmul(bg.ap[:, qd * 128:(qd + 1) * 128], lhsT=sel, rhs=g2,
                                                                                 start=True, stop=True),
                              deps=([pg, pb_] + bg.free + allconst) if qd == 0 else (), signal=(qd == 3))
                for qd in range(4):
                    kc = blk * 4 + qd
                    sel = ident[0:KC, kc:kc + 1].to_broadcast([KC, 128])
                    mb = P.op("tensor", lambda e, qd=qd, sel=sel, bb=bb: e.matmul(bb.ap[:, qd * 128:(qd + 1) * 128], lhsT=sel, rhs=b2,
                                                                                 start=True, stop=True),
                              deps=([pg, pb_] + bb.free) if qd == 0 else (), signal=(qd == 3))
                l1 = l2 = None
                if _dbg < 3:
                    bg.free = [mg]
                    bb.free = [mb]
                    fin = norm_ev[-1]
                    continue
                for tt in range(4):
                    l1 = P.op("vector", lambda e, tt=tt, blk=blk, bg=bg: e.tensor_tensor(
                        out=ACC[:, tt, blk * 512:(blk + 1) * 512], in0=ACC[:, tt, blk * 512:(blk + 1) * 512], in1=bg.ap, op=ALU.mult),
                        deps=[mg, norm_ev[tt]])
                    l2 = P.op("vector", lambda e, tt=tt, blk=blk, bb=bb: e.tensor_tensor(
                        out=ACC[:, tt, blk * 512:(blk + 1) * 512], in0=ACC[:, tt, blk * 512:(blk + 1) * 512], in1=bb.ap, op=ALU.add),
                        deps=[mb, l1])
                    fin = l2
                bg.free = [l1]
                bb.free = [l2]
            for tt in range(4):
                acc_b[tt].ready = [fin]
                acc_b[tt].free = []
            vb_b.free = [mg, mb]

        def wout_phase(w_out_l, ut_ready):
            fin = None
            mm = None
            wsrc = w_out_l.rearrange("(k p) n -> p k n", p=128)
            nkh = KC // 2
            for cbk in range(D // 512):
                banks = [psget() for _ in range(4)]
                for half in range(2):
                    wb, wv = wload(wsrc[:, half * nkh:(half + 1) * nkh, cbk * 512:(cbk + 1) * 512], nkh, 512)
                    for tt in range(4):
                        pb = banks[tt]
                        for k in range(nkh):
                            kc = half * nkh + k
                            mm = P.op("tensor", lambda e, tt=tt, kc=kc, k=k, pb=pb, wv=wv: e.matmul(
                                pb.ap[:, 0:512], lhsT=UT3[:, kc, tt * 128:(tt + 1) * 128], rhs=wv[:, k, :],
                                start=(kc == 0), stop=(kc == KC - 1)),
                                deps=(wb.ready + (pb.free if half == 0 else []) + ut_ready) if k == 0 else (), signal=(k == nkh - 1))
                    wb.free = [mm]
                for tt in range(4):
                    pb = banks[tt]
                    ev = P.op("vector", lambda e, tt=tt, cbk=cbk, pb=pb: e.scalar_tensor_tensor(
                        out=ACC[:, tt, cbk * 512:(cbk + 1) * 512], in0=ACC[:, tt, cbk * 512:(cbk + 1) * 512], scalar=c.ALPHA,
                        in1=pb.ap[:, 0:512], op0=ALU.mult, op1=ALU.add), deps=[mm] + acc_b[tt].free + acc_b[tt].ready)
                    pb.free = [ev]
                    fin = ev
            for tt in range(4):
                acc_b[tt].ready = [fin]
                acc_b[tt].free = []
            ut_b.free = [mm]

        def mixer_a(l):
            w_in = a_w_in[l].rearrange("(k p) n -> p k n", p=128)
            V3 = VBb.rearrange("p (t d) -> p t d", t=4)[:, :, 0:D]
            u_last = None
            mm = None
            for cbk in range(D // 256):
                wb, wv = wload(w_in[:, :, cbk * 256:(cbk + 1) * 256], KC, 256)
                for hf in range(2):
                    j = cbk * 2 + hf
                    pb = psget()
                    for kc in range(KC):
                        mm = P.op("tensor", lambda e, kc=kc, hf=hf, pb=pb, wv=wv: e.matmul(
                            pb.ap[:, 0:TT], lhsT=wv[:, kc, hf * 128:(hf + 1) * 128], rhs=XT3[:, kc, :],
                            start=(kc == 0), stop=(kc == KC - 1)),
                            deps=(wb.ready + pb.free + xt_b.ready) if kc == 0 else (), signal=(kc == KC - 1))
                    ev = P.op("scalar", lambda e, j=j, pb=pb: e.activation(out=UT3[:, j, :], in_=pb.ap[:, 0:TT], func=AF.Gelu_apprx_tanh),
                              deps=[mm] + ut_b.free)
                    pb.free = [ev]
                    u_last = ev
                wb.free = [mm]
            ut_b.free = []
            nvb = D // 512
            st_ev = [None] * 4
            nkh = KC // 2
            for cbk in range(nvb):
                banks = [psget() for _ in range(4)]
                for half in range(2):
                    wb, wv = wload(w_in[:, half * nkh:(half + 1) * nkh, D + cbk * 512:D + (cbk + 1) * 512], nkh, 512)
                    for tt in range(4):
                        pb = banks[tt]
                        for k in range(nkh):
                            kc = half * nkh + k
                            mm = P.op("tensor", lambda e, tt=tt, kc=kc, k=k, pb=pb, wv=wv: e.matmul(
                                pb.ap[:, 0:512], lhsT=XT3[:, kc, tt * 128:(tt + 1) * 128], rhs=wv[:, k, :],
                                start=(kc == 0), stop=(kc == KC - 1)),
                                deps=(wb.ready + (pb.free if half == 0 else []) + xt_b.ready) if k == 0 else (), signal=(k == nkh - 1))
                    wb.free = [mm]
                for tt in range(4):
                    pb = banks[tt]
                    ev = P.op("scalar", lambda e, tt=tt, cbk=cbk, pb=pb: e.activation(
                        out=V3[:, tt, cbk * 512:(cbk + 1) * 512], in_=pb.ap[:, 0:512], func=AF.Gelu_apprx_tanh),
                        deps=[mm] + vb_b.free)
                    pb.free = [ev]
                    st_ev[tt] = P.op("vector", lambda e, tt=tt, cbk=cbk: e.bn_stats(
                        out=lnst[:, tt * 96 + cbk * 6:tt * 96 + (cbk + 1) * 6], in_=V3[:, tt, cbk * 512:(cbk + 1) * 512]), deps=[ev])
            last_mm = mm
            vb_b.free = []
            o2 = c.AH * 128
            WmT = XT[:, 0:o2].rearrange("p (h t) -> p h t", h=c.AH)
            BT = XT[:, o2:o2 + 2 * KC * 128].bitcast(F32).rearrange("p (j t) -> p j t", j=KC)
            p1 = P.dma("sync", lambda e: e.dma_start(out=XT[:, 0:o2], in_=wmt_d[l]), sem_p, deps=[last_mm] + allconst)
            p2 = P.dma("sync", lambda e: e.dma_start(out=XT[:, o2:o2 + 2 * KC * 128].bitcast(F32), in_=bt_d[l]), sem_p,
                       deps=[last_mm] + allconst)
            g_last = None
            sp_last = None
            for tt in range(4):
                sv = lnst[:, tt * 96:tt * 96 + nvb * 6]
                mv = small[:, tt * 4:tt * 4 + 2]
                rs = small[:, tt * 4 + 2:tt * 4 + 3]
                a1 = P.op("vector", lambda e, sv=sv, mv=mv: e.bn_aggr(out=mv, in_=sv), deps=[st_ev[tt]])
                a2a = P.op("scalar", lambda e, mv=mv, rs=rs: e.activation(out=rs, in_=mv[:, 1:2], func=AF.Sqrt, bias=epsc[:, 0:1]), deps=[a1] + allconst)
                a2 = P.op("vector", lambda e, rs=rs: e.reciprocal(out=rs, in_=rs), deps=[a2a])
                a3 = P.op("vector", lambda e, tt=tt, mv=mv, rs=rs: e.tensor_scalar(
                    out=V3[:, tt, :], in0=V3[:, tt, :], scalar1=mv[:, 0:1], scalar2=rs, op0=ALU.subtract, op1=ALU.mult), deps=[a2])
                for jq in range(KC // 4):
                    pb = psget()
                    mm = None
                    for jj in range(4):
                        j = jq * 4 + jj
                        mm = P.op("tensor", lambda e, tt=tt, j=j, jj=jj, pb=pb: e.matmul(
                            pb.ap[:, jj * 128:(jj + 1) * 128], lhsT=V3[:, tt, j * 128:(j + 1) * 128], rhs=WmT[:, j // 2, :],
                            start=True, stop=True), deps=([a3, p1, p2] + pb.free) if jj == 0 else (), signal=(jj == 3))
                    sp_last = mm
                    ev2 = None
                    for jj in range(4):
                        j = jq * 4 + jj
                        tb = sp_tmp.get()
                        ev1 = P.op("vector", lambda e, j=j, jj=jj, pb=pb, tb=tb: e.scalar_tensor_tensor(
                            out=tb.ap, in0=pb.ap[:, jj * 128:(jj + 1) * 128], scalar=lng[:, l * KC + j:l * KC + j + 1],
                            in1=BT[:, j, :], op0=ALU.mult, op1=ALU.add), deps=[mm, p1, p2] + tb.free)
                        ev2 = P.op("vector", lambda e, tt=tt, j=j, tb=tb: e.tensor_tensor(
                            out=UT3[:, j, tt * 128:(tt + 1) * 128], in0=tb.ap, in1=UT3[:, j, tt * 128:(tt + 1) * 128], op=ALU.mult),
                            deps=[ev1, u_last])
                        tb.free = [ev2]
                        g_last = ev2
                    pb.free = [ev2]
            xt_b.free = [sp_last, g_last]
            vb_b.free = [sp_last]
            return [g_last]

        def mixer_b(l):
            w_in = b_w_in[l].rearrange("(k p) n -> p k n", p=128)
            g_last = None
            last_pe = None
            for h in range(c.BH):
                par = h % 2
                base = par * 3600
                xr = [VB[:, base + q * 515:base + (q + 1) * 515] for q in range(2)]
                xc = [VB[:, base + 1030 + q * 512:base + 1030 + (q + 1) * 512] for q in range(2)]
                tb = [VB[:, base + 2054 + q * 512:base + 2054 + (q + 1) * 512] for q in range(2)]
                xcb = VBb[:, 2 * (base + 3078):2 * (base + 3078) + 1024]
                ta = [xr[q][:, 0:512] for q in range(2)]
                wby, wvy = wload(w_in[:, :, h * 256:(h + 1) * 256], KC, 256)
                mm = None
                yev = []
                for hf in range(2):
                    j = h * 2 + hf
                    pb = psget()
                    for kc in range(KC):
                        mm = P.op("tensor", lambda e, kc=kc, hf=hf, pb=pb, wvy=wvy: e.matmul(
                            pb.ap[:, 0:TT], lhsT=wvy[:, kc, hf * 128:(hf + 1) * 128], rhs=XT3[:, kc, :],
                            start=(kc == 0), stop=(kc == KC - 1)),
                            deps=(wby.ready + pb.free + xt_b.ready) if kc == 0 else (), signal=(kc == KC - 1))
                    ev = P.op("scalar", lambda e, j=j, pb=pb: e.activation(out=UT3[:, j, :], in_=pb.ap[:, 0:TT], func=AF.Gelu_apprx_tanh),
                              deps=[mm] + ut_b.free)
                    pb.free = [ev]
                    yev.append(ev)
                wby.free = [mm]
                wbx, wvx = wload(w_in[:, :, D + h * 256:D + (h + 1) * 256], KC, 256)
                xev = []
                for hf in range(2):
                    j = h * 2 + hf
                    bj = l * KC + j
                    pb = psget()
                    for kc in range(KC):
                        mm = P.op("tensor", lambda e, kc=kc, hf=hf, pb=pb, wvx=wvx: e.matmul(
                            pb.ap[:, 0:TT], lhsT=wvx[:, kc, hf * 128:(hf + 1) * 128], rhs=XT3[:, kc, :],
                            start=(kc == 0), stop=(kc == KC - 1)),
                            deps=(wbx.ready + pb.free + xt_b.ready) if kc == 0 else (), signal=(kc == KC - 1))
                    hl = halo[:, bj * 3:bj * 3 + 3]
                    e0 = P.op("scalar", lambda e, hf=hf, hl=hl, xr=xr: e.copy(out=xr[hf][:, 0:3], in_=hl),
                              deps=vbfree[par] + vb_b.free + allconst)
                    e1_ = P.op("scalar", lambda e, hf=hf, pb=pb, xr=xr: e.copy(out=xr[hf][:, 3:515], in_=pb.ap[:, 0:TT]), deps=[mm, e0])
                    e2_ = P.op("scalar", lambda e, hf=hf, hl=hl, xr=xr: e.copy(out=hl, in_=xr[hf][:, 512:515]), deps=[e1_])
                    pb.free = [e1_]

                    def cwl(k, j=j):
                        o = (l * 4 + k) * KC + j
                        return cw[:, o:o + 1]
                    vk = P.op("vector", lambda e, hf=hf, bj=bj, xr=xr, xc=xc, cwl=cwl: e.tensor_scalar(
                        out=xc[hf], in0=xr[hf][:, 0:512], scalar1=cwl(0), scalar2=cb_[:, bj:bj + 1],
                        op0=ALU.mult, op1=ALU.add), deps=[e1_, e2_])
                    for k in range(1, 4):
                        vk = P.op("vector", lambda e, hf=hf, k=k, xr=xr, xc=xc, cwl=cwl: e.scalar_tensor_tensor(
                            out=xc[hf], in0=xr[hf][:, k:k + 512], scalar=cwl(k), in1=xc[hf], op0=ALU.mult, op1=ALU.add), deps=[vk])
                    xev.append(vk)
                wbx.free = [mm]
                cbev = []
                for hf in range(2):
                    cbev.append(P.op("scalar", lambda e, hf=hf, xc=xc, xcb=xcb: e.copy(out=xcb[:, hf * 512:(hf + 1) * 512], in_=xc[hf]),
                                     deps=[xev[hf]]))
                gwb = gw_r.get()
                gwr = gwb.ap[:, 0:512].rearrange("p (k n) -> p k n", k=2)
                gwi = gwb.ap[:, 512:1024].rearrange("p (k n) -> p k n", k=2)
                g1 = P.dma("gpsimd", lambda e, gwr=gwr, h=h: e.dma_start(out=gwr, in_=b_w_r[l, h].rearrange("(k p) n -> p k n", p=128)),
                           gwb.sem, deps=gwb.free)
                g2_ = P.dma("gpsimd", lambda e, gwi=gwi, h=h: e.dma_start(out=gwi, in_=b_w_i[l, h].rearrange("(k p) n -> p k n", p=128)),
                            gwb.sem, deps=gwb.free)
                gmm = None
                for hf in range(2):
                    j = h * 2 + hf
                    bj = l * KC + j
                    pr = psget()
                    pi = psget()
                    for (pp, gw) in ((pr, gwr), (pi, gwi)):
                        for k2 in range(2):
                            gmm = P.op("tensor", lambda e, pp=pp, gw=gw, k2=k2, hf=hf, xcb=xcb: e.matmul(
                                pp.ap[:, 0:TT], lhsT=gw[:, k2, hf * 128:(hf + 1) * 128], rhs=xcb[:, k2 * 512:(k2 + 1) * 512],
                                start=(k2 == 0), stop=(k2 == 1)),
                                deps=([g1, g2_] + cbev + pp.free) if k2 == 0 else (), signal=(k2 == 1))
                    r1 = P.op("scalar", lambda e, hf=hf, pr=pr, bj=bj, ta=ta: e.activation(out=ta[hf], in_=pr.ap[:, 0:TT], func=AF.Sigmoid,
                                                                                       bias=bbr[:, bj:bj + 1]), deps=[gmm] + xev)
                    i1 = P.op("scalar", lambda e, hf=hf, pi=pi, bj=bj, tb=tb: e.activation(out=tb[hf], in_=pi.ap[:, 0:TT], func=AF.Sigmoid,
                                                                                       bias=bbi[:, bj:bj + 1]), deps=[gmm])
                    pr.free = [r1]
                    pi.free = [i1]
                    a1 = P.op("scalar", lambda e, hf=hf, bj=bj, ta=ta: e.activation(out=ta[hf], in_=ta[hf], func=AF.Exp, scale=c8[:, bj:bj + 1]),
                              deps=[r1])
                    b1 = P.op("vector", lambda e, hf=hf, tb=tb, xc=xc: e.tensor_tensor(out=tb[hf], in0=tb[hf], in1=xc[hf], op=ALU.mult),
                              deps=[i1] + cbev)
                    m1 = P.op("vector", lambda e, hf=hf, ta=ta, xc=xc: e.scalar_tensor_tensor(out=xc[hf], in0=ta[hf], scalar=-1.0, in1=ta[hf],
                                                                                          op0=ALU.mult, op1=ALU.mult), deps=[a1, b1])
                    m2 = P.op("vector", lambda e, hf=hf, xc=xc: e.tensor_scalar(out=xc[hf], in0=xc[hf], scalar1=1.0, scalar2=1e-20,
                                                                            op0=ALU.add, op1=ALU.max), deps=[m1])
                    m3 = P.op("scalar", lambda e, hf=hf, xc=xc: e.activation(out=xc[hf], in_=xc[hf], func=AF.Sqrt), deps=[m2])
                    b2 = P.op("vector", lambda e, hf=hf, tb=tb, xc=xc: e.tensor_tensor(out=tb[hf], in0=tb[hf], in1=xc[hf], op=ALU.mult), deps=[m3])
                    hs = hstate[:, bj:bj + 1]
                    s1 = P.op("vector", lambda e, hf=hf, ta=ta, tb=tb, xc=xc, hs=hs: e.tensor_tensor_scan(
                        out=xc[hf], data0=ta[hf], data1=tb[hf], initial=hs, op0=ALU.mult, op1=ALU.add), deps=[b2] + allconst)
                    s2 = P.op("vector", lambda e, hf=hf, xc=xc, hs=hs: e.tensor_copy(out=hs, in_=xc[hf][:, 511:512]), deps=[s1])
                    s3 = P.op("vector", lambda e, hf=hf, j=j, xc=xc: e.tensor_tensor(out=UT3[:, j, :], in0=xc[hf], in1=UT3[:, j, :], op=ALU.mult),
                              deps=[s2, yev[hf]])
                    g_last = s3
                gwb.free = [gmm]
                vbfree[par] = [g_last, gmm]
                last_pe = gmm
            xt_b.free = [last_pe]
            vb_b.free = [g_last, last_pe]
            vbfree[0] = []
            vbfree[1] = []
            return [g_last]

        def moe(L, rd):
            EG, NG = c.EG, c.NG
            lg = VB[:, 3072:3072 + 4 * E].rearrange("p (t e) -> p t e", t=4)
            rb = VB[:, 3072 + 4 * E:3072 + 5 * E]
            exs = VB[:, 3072 + 5 * E:3072 + 9 * E]
            GT = VB[0:E, 3584:3584 + TT]
            bgu = VB[:, 4096:4096 + E * 2 * FC].rearrange("p (e k) -> p e k", e=E)
            bdn = [VB[0:E, q * 512:(q + 1) * 512] for q in range(2)]
            bdn_free = [[], []]
            tmp = [[VB[:, 5120 + (pz * 3 + q) * 512:5120 + (pz * 3 + q + 1) * 512] for q in range(3)] for pz in range(2)]
            tmp_free = [list(vb_b.free), list(vb_b.free)]
            r0 = P.dma("sync", lambda e: e.dma_start(out=rb, in_=router_b[L].partition_broadcast(128)), sem_p3, deps=vb_b.free)
            r1 = P.dma("sync", lambda e: e.dma_start(out=VB[:, 4096:4096 + E * 2 * FC], in_=l_b_gu[L]), sem_p3, deps=vb_b.free)
            gt_ev = []
            lgfree = []
            for tt in range(4):
                lt = lg[:, tt, :]
                m8 = small[:, 16 + tt * 8:16 + tt * 8 + 8]
                q1 = P.op("vector", lambda e, tt=tt, lt=lt: e.tensor_tensor(out=lt, in0=lgbank.ap[:, tt * E:(tt + 1) * E], in1=rb, op=ALU.add),
                          deps=[r0, r1] + rd)
                lgfree.append(q1)
                q2 = P.op("vector", lambda e, lt=lt, m8=m8: e.max(out=m8, in_=lt), deps=[q1])
                nm = small[:, 48 + tt:48 + tt + 1]
                q3 = P.op("vector", lambda e, m8=m8, nm=nm: e.tensor_scalar(out=nm, in0=m8[:, 0:1], scalar1=-1.0, scalar2=None, op0=ALU.mult),
                          deps=[q2])
                ex = exs[:, tt * E:(tt + 1) * E]
                q4 = P.op("scalar", lambda e, lt=lt, ex=ex, nm=nm: e.activation(out=ex, in_=lt, func=AF.Exp, bias=nm), deps=[q3])
                q5 = P.op("vector", lambda e, lt=lt, m8=m8: e.tensor_scalar(out=lt, in0=lt, scalar1=m8[:, c.TOPK - 1:c.TOPK], scalar2=None,
                                                                        op0=ALU.is_ge), deps=[q4])
                sm = small[:, 52 + tt:52 + tt + 1]
                q6 = P.op("vector", lambda e, lt=lt, ex=ex: e.tensor_tensor(out=lt, in0=lt, in1=ex, op=ALU.mult), deps=[q5])
                q7 = P.op("vector", lambda e, lt=lt, sm=sm: e.reduce_sum(out=sm, in_=lt, axis=AX.X), deps=[q6])
                q8 = P.op("vector", lambda e, sm=sm: e.reciprocal(out=sm, in_=sm), deps=[q7])
                q9 = P.op("vector", lambda e, lt=lt, sm=sm: e.tensor_scalar(out=lt, in0=lt, scalar1=sm, scalar2=None, op0=ALU.mult), deps=[q8])
                pb = psget()
                t1 = P.op("tensor", lambda e, lt=lt, pb=pb: e.transpose(out=pb.ap[0:E, 0:128], in_=lt, identity=ident[:]), deps=[q9] + pb.free)
                t2 = P.op("scalar", lambda e, tt=tt, pb=pb: e.copy(out=GT[:, tt * 128:(tt + 1) * 128], in_=pb.ap[0:E, 0:128]), deps=[t1])
                pb.free = [t2]
                gt_ev.append(t2)
                s0 = P.op("scalar", lambda e, tt=tt: e.mul(out=ACC[:, tt, :], in_=ACC[:, tt, :], mul=c.ALPHA), deps=rd + acc_b[tt].ready)
                acc_b[tt].ready = [s0]
            lgbank.free = lgfree
            H3 = UT[:, 0:EG * FC * TT].rearrange("p (k t) -> p k t", k=EG * FC)
            hdn_free = list(ut_b.free)
            fin = None
            pz = 0
            dmm = None
            wd = ex_w_down[L].rearrange("e (k p) n -> p (e k) n", p=128)
            nk = EG * FC
            for g in range(NG):
                h_last = None
                for ei in range(EG):
                    e_ = g * EG + ei
                    gb = psget()
                    sel = ident[0:E, e_:e_ + 1].to_broadcast([E, 128])
                    gm = P.op("tensor", lambda e, gb=gb, sel=sel: e.matmul(gb.ap[:, 0:TT], lhsT=sel, rhs=GT, start=True, stop=True),
                              deps=gt_ev + gb.free + allconst)
                    wsrc = ex_w_gu[L][e_].rearrange("(k p) n -> p k n", p=128)
                    banks = {}
                    for cbk in range(FC):
                        wb, wv = wload(wsrc[:, :, cbk * 256:(cbk + 1) * 256], KC, 256)
                        mm = None
                        for hf in range(2):
                            ch = cbk * 2 + hf
                            pb = psget()
                            for kc in range(KC):
                                mm = P.op("tensor", lambda e, kc=kc, hf=hf, pb=pb, wv=wv: e.matmul(
                                    pb.ap[:, 0:TT], lhsT=wv[:, kc, hf * 128:(hf + 1) * 128], rhs=XT3[:, kc, :],
                                    start=(kc == 0), stop=(kc == KC - 1)),
                                    deps=(wb.ready + pb.free + xt_b.ready) if kc == 0 else (), signal=(kc == KC - 1))
                            banks[ch] = (pb, mm)
                        wb.free = [mm]
                    for cc in range(FC):
                        pg_, mg_ = banks[cc]
                        pu_, mu_ = banks[FC + cc]
                        t0, t1_, t2_ = tmp[pz]
                        w0 = P.op("vector", lambda e, pg_=pg_, t0=t0, e_=e_, cc=cc: e.tensor_scalar(
                            out=t0, in0=pg_.ap[:, 0:TT], scalar1=bgu[:, e_, cc:cc + 1], scalar2=7.0, op0=ALU.add, op1=ALU.min),
                            deps=[mg_, r0, r1] + tmp_free[pz])
                        pg_.free = [w0]
                        w1 = P.op("scalar", lambda e, t0=t0, t1_=t1_: e.activation(out=t1_, in_=t0, func=AF.Sigmoid, scale=1.702), deps=[w0])
                        w2 = P.op("vector", lambda e, pu_=pu_, t2_=t2_, e_=e_, cc=cc: e.tensor_scalar(
                            out=t2_, in0=pu_.ap[:, 0:TT], scalar1=bgu[:, e_, FC + cc:FC + cc + 1], scalar2=7.0, op0=ALU.add, op1=ALU.min),
                            deps=[mu_, w0])
                        pu_.free = [w2]
                        w3 = P.op("vector", lambda e, t2_=t2_: e.tensor_scalar(out=t2_, in0=t2_, scalar1=-7.0, scalar2=1.0, op0=ALU.max, op1=ALU.add),
                                  deps=[w2])
                        w4 = P.op("vector", lambda e, t0=t0, t1_=t1_: e.tensor_tensor(out=t0, in0=t0, in1=t1_, op=ALU.mult), deps=[w1, w3])
                        w5 = P.op("vector", lambda e, t0=t0, t2_=t2_: e.tensor_tensor(out=t0, in0=t0, in1=t2_, op=ALU.mult), deps=[w4])
                        w6 = P.op("vector", lambda e, t0=t0, gb=gb, ei=ei, cc=cc: e.tensor_tensor(
                            out=H3[:, ei * FC + cc, :], in0=t0, in1=gb.ap[:, 0:TT], op=ALU.mult), deps=[w5, gm] + hdn_free)
                        tmp_free[pz] = [w6]
                        pz ^= 1
                        h_last = w6
                    gb.free = [h_last]
                hdn_free = []
                nkh2 = nk // 2
                for cbk in range(D // 512):
                    bq = cbk % 2
                    bl = None
                    if g == 0:
                        bl = P.dma("sync", lambda e, bq=bq, cbk=cbk: e.dma_start(out=bdn[bq], in_=ex_b_down[L][:, cbk * 512:(cbk + 1) * 512]),
                                   bsem[bq], deps=bdn_free[bq] + [r0, r1] + rd)
                    banks = [psget() for _ in range(4)]
                    for half in range(2):
                        wb, wv = wload(wd[:, g * nk + half * nkh2:g * nk + (half + 1) * nkh2, cbk * 512:(cbk + 1) * 512], nkh2, 512)
                        for tt in range(4):
                            pb = banks[tt]
                            if g == 0 and half == 0:
                                P.op("tensor", lambda e, tt=tt, pb=pb, bq=bq: e.matmul(pb.ap[:, 0:512], lhsT=GT[:, tt * 128:(tt + 1) * 128], rhs=bdn[bq],
                                                                                       start=True, stop=False),
                                     deps=[bl] + gt_ev + pb.free, signal=False)
                            for k in range(nkh2):
                                kk = half * nkh2 + k
                                dmm = P.op("tensor", lambda e, tt=tt, k=k, kk=kk, pb=pb, wv=wv, g=g: e.matmul(
                                    pb.ap[:, 0:512], lhsT=H3[:, kk, tt * 128:(tt + 1) * 128], rhs=wv[:, k, :],
                                    start=(kk == 0 and g != 0), stop=(kk == nk - 1)),
                                    deps=(wb.ready + [h_last] + (pb.free if (g != 0 and half == 0) else [])) if k == 0 else (),
                                    signal=(k == nkh2 - 1))
                        wb.free = [dmm]
                    for tt in range(4):
                        pb = banks[tt]
                        ev = P.op("vector", lambda e, tt=tt, cbk=cbk, pb=pb: e.tensor_tensor(
                            out=ACC[:, tt, cbk * 512:(cbk + 1) * 512], in0=ACC[:, tt, cbk * 512:(cbk + 1) * 512], in1=pb.ap[:, 0:512], op=ALU.add),
                            deps=[dmm] + acc_b[tt].ready)
                        pb.free = [ev]
                        fin = ev
                    if g == 0:
                        bdn_free[bq] = [dmm]
                hdn_free = [dmm]
            ut_b.free = [dmm]
            xt_b.free = [dmm]
            for tt in range(4):
                acc_b[tt].ready = [fin]
                acc_b[tt].free = []
            vb_b.free = [dmm, fin]

        out_ev = []
        for s in range(NST):
            for tt in range(4):
                r0_ = s * TT + tt * 128
                ev = P.dma("sync", lambda e, tt=tt, r0_=r0_: e.dma_start(out=ACC[:, tt, :], in_=x[r0_:r0_ + 128, :]), sem_in[tt],
                           deps=acc_b[tt].free + acc_b[tt].ready)
                acc_b[tt].ready = [ev]
                acc_b[tt].free = []
            for L in layers:
                l = L // 2
                cur_layer[0] = L
                if upto < 1:
                    break
                build_xt()
                if upto < 2:
                    break
                if L % 2 == 0:
                    ut_ready = mixer_a(l)
                else:
                    ut_ready = mixer_b(l)
                if upto < 3:
                    break
                wout_phase(a_w_out[l] if L % 2 == 0 else b_w_out[l], ut_ready)
                if upto < 4:
                    break
                layer_norm(ln1_g[L], ln1_b[L])
                if upto < 5:
                    break
                rd = build_xt(router_layer=L)
                if upto < 6:
                    break
                moe(L, rd)
                if upto < 7:
                    break
                layer_norm(ln2_g[L], ln2_b[L])
            for tt in range(4):
                r0_ = s * TT + tt * 128
                ev = P.dma("sync", lambda e, tt=tt, r0_=r0_: e.dma_start(out=out[r0_:r0_ + 128, :], in_=ACC[:, tt, :]), sem_out[tt],
                           deps=acc_b[tt].ready)
                acc_b[tt].free = [ev]
                out_ev.append(ev)
        P.wait_only("sync", out_ev)
        with nc.Block() as block:
            P.replay(block)
    return nc, P


def _pj(v, KC):
    v = np.asarray(v, np.float32)
    lead = int(np.prod(v.shape[:-1])) if v.ndim > 1 else 1
    v = v.reshape(lead, KC, 128)
    return np.ascontiguousarray(v.transpose(2, 0, 1).reshape(128, lead * KC))


def prep_inputs(cfg, inputs):
    c = cfg
    KC, E, FC = c.KC, c.E, c.FC
    f = lambda k: np.ascontiguousarray(inputs[k], dtype=np.float32)
    m = {}
    for k in ["a_w_in", "a_w_out", "b_w_in", "b_w_r", "b_w_i", "b_w_out", "ln1_g", "ln1_b", "ln2_g", "ln2_b",
              "router_b", "ex_w_gu", "ex_w_down", "ex_b_down", "a_b_s"]:
        m[k] = f(k)
    m["l_a_ln_g"] = _pj(f("a_ln_g"), KC)
    m["l_a_ln_b"] = _pj(f("a_ln_b"), KC)
    m["l_a_w_sT"] = np.ascontiguousarray(f("a_w_s").transpose(0, 3, 1, 2).reshape(c.nA, 128, c.AH * 128))
    m["l_conv_w"] = _pj(f("b_conv_w"), KC)
    m["l_conv_b"] = _pj(f("b_conv_b"), KC)
    m["l_b_r"] = _pj(f("b_b_r").reshape(c.nB, -1), KC)
    m["l_b_i"] = _pj(f("b_b_i").reshape(c.nB, -1), KC)
    m["l_lam"] = _pj(f("b_lambda"), KC)
    rw = f("router_w").reshape(c.DEPTH, KC, 128, E)
    m["l_router_w"] = np.ascontiguousarray(rw.transpose(0, 2, 1, 3).reshape(c.DEPTH, 128, KC * E))
    bg = f("ex_b_gu").reshape(c.DEPTH, E, 2 * FC, 128)
    m["l_b_gu"] = np.ascontiguousarray(bg.transpose(0, 3, 1, 2).reshape(c.DEPTH, 128, E * 2 * FC))
    m["c_ident"] = np.eye(128, dtype=np.float32)
    m["c_triu"] = np.triu(np.ones((128, 128), np.float32))
    return m


def run(cfg, inputs, layers=None, trace=False, upto=99):
    nc, P = build_program(cfg, layers, upto)
    x = np.ascontiguousarray(inputs["x"], dtype=np.float32)
    xs = x.reshape(-1, cfg.D)
    n = cfg.n_cores
    per = xs.shape[0] // n
    assert per == cfg.NTOK
    base = prep_inputs(cfg, inputs)
    in_maps = []
    for ci in range(n):
        m = dict(base)
        m["x"] = xs[ci * per:(ci + 1) * per]
        in_maps.append(m)
    res = run_bass_kernel_spmd(nc, in_maps, core_ids=list(range(n)), trace=trace)
    y = np.concatenate([r["out"] for r in res.results], axis=0).reshape(x.shape)
    return y, res


def kernel(**inputs):
    cfg = Cfg()
    y, _ = run(cfg, inputs)
    return y.astype(np.float32)
```

```python
import math
import numpy as np
from contextlib import ExitStack
import concourse.bass as bass
import concourse.mybir as mybir
from concourse.bass_utils import run_bass_kernel_spmd

F32 = mybir.dt.float32
BF16 = mybir.dt.bfloat16
AF = mybir.ActivationFunctionType
ALU = mybir.AluOpType
AX = mybir.AxisListType

ENGS = ["sync", "scalar", "vector", "gpsimd", "tensor"]
SEM_LIMIT = 30000


class Cfg:
    def __init__(self, D=4096, NTOK=8192, DEPTH=4, AH=16, BH=16, E=32, F=384, TOPK=4, n_cores=2):
        self.D = D
        self.KC = D // 128
        self.NTOK = NTOK
        self.TT = 512
        self.NST = NTOK // 512
        self.DEPTH = DEPTH
        self.AH = AH
        self.BH = BH
        self.E = E
        self.F = F
        self.FC = F // 128
        self.TOPK = TOPK
        self.n_cores = n_cores
        self.nA = (DEPTH + 1) // 2
        self.nB = DEPTH // 2
        self.ALPHA = float((2 * DEPTH) ** 0.25)
        self.EPS = 1e-5
        self.EG = min(8, E)
        self.NG = E // self.EG


class Ev:
    __slots__ = ("sem", "val")

    def __init__(self, sem, val):
        self.sem = sem
        self.val = val


def _flat(deps):
    out = []
    if deps is None:
        return out
    if isinstance(deps, Ev):
        return [deps]
    for d in deps:
        if d is None:
            continue
        if isinstance(d, Ev):
            out.append(d)
        else:
            out.extend(_flat(d))
    return out


class Prog:
    def __init__(self, nc, stack):
        self.nc = nc
        self.stack = stack
        self.q = {e: [] for e in ENGS}
        self.cur = {}
        self.waited = {e: {} for e in ENGS}
        self.nsem = 0
        self.ninst = 0

    def new_sem(self, name):
        self.nsem += 1
        return self.stack.enter_context(self.nc.semaphore(f"{name}_{self.nsem}"))

    def _eng_sem(self, eng):
        c = self.cur.get(eng)
        if c is None or c[1] >= SEM_LIMIT:
            c = [self.new_sem("e" + eng), 0]
            self.cur[eng] = c
        return c

    def _waits(self, eng, deps):
        wd = self.waited[eng]
        best = {}
        for d in _flat(deps):
            k = id(d.sem)
            if wd.get(k, 0) >= d.val:
                continue
            if k not in best or best[k][1] < d.val:
                best[k] = (d.sem, d.val)
        w = []
        for k, (sem, val) in best.items():
            wd[k] = val
            w.append((sem, val))
        return w

    def op(self, eng, fn, deps=(), signal=True):
        waits = self._waits(eng, deps)
        ev = None
        inc = None
        if signal:
            c = self._eng_sem(eng)
            c[1] += 1
            ev = Ev(c[0], c[1])
            inc = (c[0], 1)
        self.q[eng].append((fn, waits, inc))
        self.ninst += 1
        return ev

    def dma(self, eng, fn, sem_state, deps=()):
        waits = self._waits(eng, deps)
        sem_state[1] += 16
        ev = Ev(sem_state[0], sem_state[1])
        self.q[eng].append((fn, waits, (sem_state[0], 16)))
        self.ninst += 1
        return ev

    def dsem(self, name="d"):
        return [self.new_sem(name), 0]

    def wait_only(self, eng, deps):
        waits = self._waits(eng, deps)
        if waits:
            self.q[eng].append((None, waits, None))

    def replay(self, block):
        for eng in ENGS:
            items = self.q[eng]
            if not items:
                continue

            def body(e, items=items):
                for fn, waits, inc in items:
                    for (s, v) in waits:
                        e.wait_ge(s, v)
                    if fn is None:
                        continue
                    ins = fn(e)
                    if inc is not None:
                        ins.then_inc(inc[0], inc[1])

            getattr(block, eng)(body)


class Buf:
    def __init__(self, ap):
        self.ap = ap
        self.ready = []
        self.free = []


class Ring:
    def __init__(self, aps):
        self.bufs = [Buf(a) for a in aps]
        self.i = 0

    def get(self):
        b = self.bufs[self.i % len(self.bufs)]
        self.i += 1
        return b


def build_program(cfg, layers=None, upto=99):
    c = cfg
    D, KC, TT, NST, E, FC = c.D, c.KC, c.TT, c.NST, c.E, c.FC
    layers = list(range(c.DEPTH)) if layers is None else layers
    nc = bass.Bass("TRN2", target_bir_lowering=False)

    def din(name, shape):
        return nc.dram_tensor(name, list(shape), F32, kind="ExternalInput").ap()

    nA1, nB1 = max(c.nA, 1), max(c.nB, 1)
    x = din("x", [c.NTOK, D])
    a_w_in = din("a_w_in", [c.nA, D, 2 * D])
    a_w_out = din("a_w_out", [c.nA, D, D])
    b_w_in = din("b_w_in", [c.nB, D, 2 * D])
    b_w_r = din("b_w_r", [c.nB, c.BH, 256, 256])
    b_w_i = din("b_w_i", [c.nB, c.BH, 256, 256])
    b_w_out = din("b_w_out", [c.nB, D, D])
    ln1_g = din("ln1_g", [c.DEPTH, D])
    ln1_b = din("ln1_b", [c.DEPTH, D])
    ln2_g = din("ln2_g", [c.DEPTH, D])
    ln2_b = din("ln2_b", [c.DEPTH, D])
    router_b = din("router_b", [c.DEPTH, E])
    ex_w_gu = din("ex_w_gu", [c.DEPTH, E, D, 2 * c.F])
    ex_w_down = din("ex_w_down", [c.DEPTH, E, c.F, D])
    ex_b_down = din("ex_b_down", [c.DEPTH, E, D])
    a_b_s = din("a_b_s", [c.nA, c.AH, 128])
    l_a_ln_g = din("l_a_ln_g", [128, nA1 * KC])
    l_a_ln_b = din("l_a_ln_b", [128, nA1 * KC])
    l_a_w_sT = din("l_a_w_sT", [nA1, 128, c.AH * 128])
    l_conv_w = din("l_conv_w", [128, nB1 * 4 * KC])
    l_conv_b = din("l_conv_b", [128, nB1 * KC])
    l_b_r = din("l_b_r", [128, nB1 * KC])
    l_b_i = din("l_b_i", [128, nB1 * KC])
    l_lam = din("l_lam", [128, nB1 * KC])
    l_router_w = din("l_router_w", [c.DEPTH, 128, KC * E])
    l_b_gu = din("l_b_gu", [c.DEPTH, 128, E * 2 * FC])
    c_ident = din("c_ident", [128, 128])
    c_triu = din("c_triu", [128, 128])
    out = nc.dram_tensor("out", [c.NTOK, D], F32, kind="ExternalOutput").ap()
    big = {"a_w_in": a_w_in, "a_w_out": a_w_out, "b_w_in": b_w_in, "b_w_out": b_w_out,
           "ex_w_gu": ex_w_gu, "ex_w_down": ex_w_down}
    bigb = {}
    for nm_, ap_ in big.items():
        bigb[nm_] = [nc.dram_tensor(f"{nm_}_bf{i_}", list(ap_.shape[1:]), BF16).ap() for i_ in range(ap_.shape[0])]
    wmt_d = nc.dram_tensor("wmt_d", [nA1, 128, c.AH * 128], BF16).ap()
    bt_d = nc.dram_tensor("bt_d", [nA1, 128, KC * 128], F32).ap()

    st = ExitStack()
    with st:
        P = Prog(nc, st)

        def sb(name, shape, dt):
            return st.enter_context(nc.sbuf_tensor(name, list(shape), dt))

        ACC = sb("ACC", [128, 4, D], F32)
        XT = sb("XT", [128, KC * TT], BF16)
        UT = sb("UT", [128, max(KC * TT, c.EG * FC * TT)], BF16)
        VB = sb("VB", [128, 8192], F32)
        WR = [sb(f"WR{i}", [128, max(KC, c.EG * FC) * 256], BF16) for i in range(2)]
        ident = sb("ident", [128, 128], F32)
        XT3 = XT[:].rearrange("p (k t) -> p k t", k=KC)
        UT3 = UT[:, 0:KC * TT].rearrange("p (k t) -> p k t", k=KC)
        VBb = VB[:].bitcast(BF16)

        lng = sb("lng", [128, nA1 * KC], F32)
        lnb = sb("lnb", [128, nA1 * KC], F32)
        cw = sb("cw", [128, nB1 * 4 * KC], F32)
        cb_ = sb("cb", [128, nB1 * KC], F32)
        bbr = sb("bbr", [128, nB1 * KC], F32)
        bbi = sb("bbi", [128, nB1 * KC], F32)
        c8 = sb("c8", [128, nB1 * KC], F32)
        hstate = sb("hstate", [128, nB1 * KC], F32)
        halo = sb("halo", [128, nB1 * KC * 3], F32)
        small = sb("small", [128, 64], F32)
        epsc = sb("epsc", [128, 1], F32)
        lnst = sb("lnst", [128, 4 * 96], F32)
        sp_tmp_t = sb("sp_tmp", [128, 2 * 128], F32)
        gw_t0 = sb("gwt", [128, 2 * 1024], BF16)

        wring = Ring([w[:] for w in WR])
        wsem = [P.dsem("w0"), P.dsem("w1")]
        PSB = [st.enter_context(nc.psum_tensor(f"ps{i}", [128, 512], F32)) for i in range(8)]
        psring = Ring([p[:] for p in PSB[0:7]])
        lgbank = Buf(PSB[7][:])

        acc_b = [Buf(ACC[:, tt, :]) for tt in range(4)]
        xt_b = Buf(XT3)
        ut_b = Buf(UT3)
        vb_b = Buf(VB[:])
        sem_in = [P.dsem(f"in{i}") for i in range(4)]
        sem_out = [P.dsem(f"out{i}") for i in range(4)]
        sem_c = P.dsem("const")
        sem_pr = P.dsem("prep")
        sem_p = P.dsem("par")
        sem_p2 = P.dsem("par2")
        sem_p3 = P.dsem("par3")
        bsem = [P.dsem("bd0"), P.dsem("bd1")]
        sp_tmp = Ring([sp_tmp_t[:, 0:128], sp_tmp_t[:, 128:256]])
        gw_r = Ring([gw_t0[:, 0:1024], gw_t0[:, 1024:2048]])
        for i_, b_ in enumerate(gw_r.bufs):
            b_.sem = P.dsem(f"gw{i_}")
        vbfree = [[], []]

        def wload(src3, nk, ncols):
            slot = wring.i % 2
            b = wring.get()
            view = b.ap[:, 0:nk * ncols].rearrange("p (k n) -> p k n", k=nk)
            Lc = cur_layer[0]
            emit_conv(Lc, 10 ** 9)
            ev = P.dma("gpsimd", lambda e, view=view, src3=src3: e.dma_start(out=view, in_=src3), wsem[slot],
                       deps=b.free + [cvt_done[Lc]])
            nxt = layers[(layers.index(Lc) + 1) % len(layers)]
            emit_conv(nxt, 1)
            b.free = []
            b.ready = [ev]
            return b, view

        def psget():
            return psring.get()

        CH = 8192
        conv_list = {L_: [] for L_ in range(c.DEPTH)}

        def _add_conv(L_, src_, dst_):
            tot = int(np.prod(src_.shape))
            names = " ".join(f"d{i}" for i in range(len(src_.shape)))
            sf = src_.rearrange(f"{names} -> ({names})").rearrange("(p n) -> p n", p=128)
            df = dst_.rearrange(f"{names} -> ({names})").rearrange("(p n) -> p n", p=128)
            per = tot // 128
            for o in range(0, per, CH):
                w_ = min(CH, per - o)
                conv_list[L_].append(lambda e, sf=sf, df=df, o=o, w_=w_: e.dma_start(out=df[:, o:o + w_], in_=sf[:, o:o + w_]))

        for L_ in range(c.DEPTH):
            l_ = L_ // 2
            if L_ % 2 == 0:
                _add_conv(L_, big["a_w_in"][l_], bigb["a_w_in"][l_])
                _add_conv(L_, big["a_w_out"][l_], bigb["a_w_out"][l_])
            else:
                _add_conv(L_, big["b_w_in"][l_], bigb["b_w_in"][l_])
                _add_conv(L_, big["b_w_out"][l_], bigb["b_w_out"][l_])
            _add_conv(L_, big["ex_w_gu"][L_], bigb["ex_w_gu"][L_])
            _add_conv(L_, big["ex_w_down"][L_], bigb["ex_w_down"][L_])
        sem_cvL = [P.dsem(f"cvt{L_}") for L_ in range(c.DEPTH)]
        cvt_done = [Ev(sem_cvL[L_][0], 16 * len(conv_list[L_])) for L_ in range(c.DEPTH)]
        conv_pos = [0] * c.DEPTH
        cur_layer = [0]

        def emit_conv(L_, n):
            while n > 0 and conv_pos[L_] < len(conv_list[L_]):
                P.dma("gpsimd", conv_list[L_][conv_pos[L_]], sem_cvL[L_])
                conv_pos[L_] += 1
                n -= 1

        emit_conv(layers[0], 10 ** 9)
        a_w_in, a_w_out, b_w_in, b_w_out, ex_w_gu, ex_w_down = (bigb[k_] for k_ in
                                                                  ["a_w_in", "a_w_out", "b_w_in", "b_w_out", "ex_w_gu", "ex_w_down"])

        cev = [P.dma("sync", lambda e: e.dma_start(out=ident[:], in_=c_ident[:, :]), sem_c)]

        def cload(dst, src):
            cev.append(P.dma("sync", lambda e: e.dma_start(out=dst, in_=src), sem_c))

        cload(lng[:], l_a_ln_g[:, :])
        cload(lnb[:], l_a_ln_b[:, :])
        cload(cw[:], l_conv_w[:, :])
        cload(cb_[:], l_conv_b[:, :])
        cload(bbr[:], l_b_r[:, :])
        cload(bbi[:], l_b_i[:, :])
        cload(c8[:], l_lam[:, :])
        const_ev = list(cev)
        e1 = P.op("scalar", lambda e: e.activation(out=c8[:], in_=c8[:], func=AF.Exp, scale=-1.0), deps=cev)
        e2 = P.op("scalar", lambda e: e.activation(out=c8[:], in_=c8[:], func=AF.Ln, bias=1.0), deps=[e1])
        e3 = P.op("scalar", lambda e: e.mul(out=c8[:], in_=c8[:], mul=-8.0), deps=[e2])
        e4 = P.op("vector", lambda e: e.memset(hstate[:], 0.0))
        e5 = P.op("vector", lambda e: e.memset(halo[:], 0.0), deps=[e4])
        e6 = P.op("vector", lambda e: e.memset(epsc[:], c.EPS), deps=[e5])
        const_ev += [e3, e4, e5, e6]

        prep_ev = []
        AHn = c.AH
        for l in range(c.nA):
            WT32 = ACC[:, 0, 0:AHn * 128].rearrange("p (h t) -> p h t", h=AHn)
            WmTb = VBb[:, 0:AHn * 128].rearrange("p (h t) -> p h t", h=AHn)
            bsbc = ACC[:, 1, 0:AHn * 128].rearrange("p (h t) -> p h t", h=AHn)
            BTt = ACC[:, 2, 0:KC * 128].rearrange("p (j t) -> p j t", j=KC)
            triu = VB[:, 4096:4096 + 128]
            ones = VB[:, 4096 + 128:4096 + 256]
            d0 = list(prep_ev)
            l1 = P.dma("sync", lambda e, l=l: e.dma_start(out=ACC[:, 0, 0:AHn * 128], in_=l_a_w_sT[l]), sem_pr, deps=d0)
            l2 = P.dma("sync", lambda e, triu=triu: e.dma_start(out=triu, in_=c_triu[:, :]), sem_pr, deps=d0)
            l3 = P.dma("sync", lambda e, l=l: e.dma_start(out=ACC[:, 1, 0:AHn * 128],
                                                          in_=a_b_s[l].rearrange("h t -> (h t)").partition_broadcast(128)),
                       sem_pr, deps=d0)
            m0 = P.op("vector", lambda e, ones=ones: e.memset(ones, 1.0), deps=d0 + const_ev)
            mk = m0
            for h in range(AHn):
                mk = P.op("vector", lambda e, h=h, WT32=WT32, triu=triu: e.tensor_tensor(out=WT32[:, h, :], in0=WT32[:, h, :], in1=triu, op=ALU.mult),
                          deps=[l1, l2, l3, mk])
            cvt = P.op("vector", lambda e, WT32=WT32, WmTb=WmTb: e.tensor_copy(out=WmTb, in_=WT32), deps=[mk])
            bev = []
            for q in range((AHn * 128 + 511) // 512):
                pb = psget()
                ncol = min(512, AHn * 128 - q * 512)
                mm = P.op("tensor", lambda e, q=q, pb=pb, ncol=ncol, ones=ones: e.matmul(
                    pb.ap[:, 0:ncol], lhsT=ones, rhs=ACC[:, 0, q * 512:q * 512 + ncol], start=True, stop=True),
                    deps=[mk, m0] + pb.free)
                rl = []
                for hh in range(ncol // 128):
                    h = q * 4 + hh
                    for jj in range(2):
                        j = h * 2 + jj
                        rl.append(P.op("vector", lambda e, pb=pb, hh=hh, j=j, h=h, l=l, BTt=BTt, bsbc=bsbc: e.scalar_tensor_tensor(
                            out=BTt[:, j, :], in0=pb.ap[:, hh * 128:(hh + 1) * 128], scalar=lnb[:, l * KC + j:l * KC + j + 1],
                            in1=bsbc[:, h, :], op0=ALU.mult, op1=ALU.add), deps=[mm, l3] + const_ev))
                pb.free = rl
                bev += rl
            s1 = P.dma("sync", lambda e, l=l: e.dma_start(out=wmt_d[l], in_=VBb[:, 0:AHn * 128]), sem_pr, deps=[cvt])
            s2 = P.dma("sync", lambda e, l=l: e.dma_start(out=bt_d[l], in_=ACC[:, 2, 0:KC * 128]), sem_pr, deps=bev)
            prep_ev = [s1, s2]
        for b in acc_b:
            b.free = list(prep_ev)
        vb_b.free = list(prep_ev)
        allconst = const_ev + prep_ev

        def build_xt(router_layer=None):
            evs_all = []
            rd = []
            if router_layer is not None:
                L = router_layer
                rw = VB[:, 0:KC * E].rearrange("p (k e) -> p k e", k=KC)
                rwev = P.dma("sync", lambda e: e.dma_start(out=VB[:, 0:KC * E], in_=l_router_w[L]), sem_p, deps=vb_b.free)
                stg_r = Ring([VB[:, 2048:2560], VB[:, 2560:3072]])
                for sgb in stg_r.bufs:
                    sgb.free = list(vb_b.free)
            for tt in range(4):
                for q in range(KC // 4):
                    pb = psget()
                    tl = None
                    for i in range(4):
                        kc = q * 4 + i
                        tl = P.op("tensor", lambda e, tt=tt, kc=kc, i=i, pb=pb: e.transpose(
                            out=pb.ap[:, i * 128:(i + 1) * 128], in_=ACC[:, tt, kc * 128:(kc + 1) * 128], identity=ident[:]),
                            deps=(acc_b[tt].ready + pb.free + allconst) if i == 0 else (), signal=(i == 3))
                    ce = P.op("scalar", lambda e, tt=tt, q=q, pb=pb: e.copy(
                        out=XT3[:, q * 4:(q + 1) * 4, tt * 128:(tt + 1) * 128],
                        in_=pb.ap.rearrange("p (k t) -> p k t", k=4)), deps=[tl] + xt_b.free)
                    fr = [ce]
                    if router_layer is not None:
                        sg = stg_r.get()
                        c2 = P.op("vector", lambda e, pb=pb, sg=sg: e.tensor_copy(out=sg.ap, in_=pb.ap), deps=[tl, ce] + sg.free)
                        fr.append(c2)
                        ml = None
                        for i in range(4):
                            kc = q * 4 + i
                            ml = P.op("tensor", lambda e, i=i, kc=kc, sg=sg, tt=tt, rw=rw: e.matmul(
                                lgbank.ap[:, tt * E:(tt + 1) * E], lhsT=sg.ap[:, i * 128:(i + 1) * 128], rhs=rw[:, kc, :],
                                start=(kc == 0), stop=(kc == KC - 1)),
                                deps=([c2, rwev] + (lgbank.free if (kc == 0 and tt == 0) else [])) if i == 0 else (), signal=(i == 3))
                        sg.free = [ml]
                        rd.append(ml)
                    pb.free = fr
                    evs_all.append(ce)
                    rd.append(tl)
            xt_b.free = []
            xt_b.ready = evs_all[-1:]
            for tt in range(4):
                acc_b[tt].free = acc_b[tt].free + rd[-2:]
            return rd[-2:]

        def layer_norm(gsrc, bsrc):
            g2 = VB[0:KC, 7168:7168 + 128]
            b2 = VB[0:KC, 7168 + 128:7168 + 256]
            import os as _os
            _dbg = int(_os.environ.get("LNDBG", "9"))
            pg = pb_ = None
            if _dbg != 0:
                pg = P.dma("sync", lambda e: e.dma_start(out=g2, in_=gsrc.rearrange("(k c) -> k c", c=128)), sem_p2, deps=vb_b.free)
                pb_ = P.dma("sync", lambda e: e.dma_start(out=b2, in_=bsrc.rearrange("(k c) -> k c", c=128)), sem_p2, deps=vb_b.free)
            nblk = D // 512
            norm_ev = []
            for tt in range(4):
                sv = lnst[:, tt * 96:(tt + 1) * 96]
                se = None
                _lnv = int(_os.environ.get("LNV", "0"))
                for blk in range(nblk):
                    if _lnv == 2:
                        se = P.op("vector", lambda e, tt=tt, blk=blk, sv=sv: e.tensor_copy(
                            out=sv[:, blk * 6:(blk + 1) * 6], in_=ACC[:, tt, blk * 512:blk * 512 + 6]),
                            deps=acc_b[tt].ready if blk == 0 else (), signal=(blk == nblk - 1))
                        continue
                    se = P.op("vector", lambda e, tt=tt, blk=blk, sv=sv: e.bn_stats(
                        out=sv[:, blk * 6:(blk + 1) * 6], in_=ACC[:, tt, blk * 512:(blk + 1) * 512]),
                        deps=(acc_b[tt].ready if _lnv != 1 else ()) if blk == 0 else (), signal=(blk == nblk - 1))
                mv = small[:, tt * 4:tt * 4 + 2]
                rs = small[:, tt * 4 + 2:tt * 4 + 3]
                _sub = int(_os.environ.get("LNSUB", "9"))
                if _sub < 2:
                    norm_ev.append(se); continue
                a1 = P.op("vector", lambda e, sv=sv, mv=mv: e.bn_aggr(out=mv, in_=sv[:, 0:nblk * 6]), deps=[se])
                if _sub < 3:
                    norm_ev.append(a1); continue
                a2a = P.op("scalar", lambda e, mv=mv, rs=rs: e.activation(out=rs, in_=mv[:, 1:2], func=AF.Sqrt, bias=epsc[:, 0:1]), deps=[a1] + allconst)
                if _sub < 4:
                    norm_ev.append(a2a); continue
                a2 = P.op("vector", lambda e, rs=rs: e.reciprocal(out=rs, in_=rs), deps=[a2a])
                if _sub < 5:
                    norm_ev.append(a2); continue
                a3 = P.op("vector", lambda e, tt=tt, mv=mv, rs=rs: e.tensor_scalar(
                    out=ACC[:, tt, :], in0=ACC[:, tt, :], scalar1=mv[:, 0:1], scalar2=rs, op0=ALU.subtract, op1=ALU.mult),
                    deps=[a2] + acc_b[tt].free)
                norm_ev.append(a3)
            fin = None
            mg = mb = None
            import os as _os
            _dbg = int(_os.environ.get("LNDBG", "9"))
            if _dbg < 2:
                for tt in range(4):
                    acc_b[tt].ready = [norm_ev[-1]]
                    acc_b[tt].free = []
                vb_b.free = [pg, pb_]
                return
            for blk in range(nblk):
                bg = psget()
                bb = psget()
                for qd in range(4):
                    kc = blk * 4 + qd
                    sel = ident[0:KC, kc:kc + 1].to_broadcast([KC, 128])
                    mg = P.op("tensor", lambda e, qd=qd, sel=sel, bg=bg: e.matmul(bg.ap[:, qd * 128:(qd + 1) * 128], lhsT=sel, rhs=g2,
                                                                                 start=True, stop=True),
                              deps=([pg, pb_] + bg.free + allconst) if qd == 0 else (), signal=(qd == 3))
                for qd in range(4):
                    kc = blk * 4 + qd
                    sel = ident[0:KC, kc:kc + 1].to_broadcast([KC, 128])
                    mb = P.op("tensor", lambda e, qd=qd, sel=sel, bb=bb: e.matmul(bb.ap[:, qd * 128:(qd + 1) * 128], lhsT=sel, rhs=b2,
                                                                                 start=True, stop=True),
                              deps=([pg, pb_] + bb.free) if qd == 0 else (), signal=(qd == 3))
                l1 = l2 = None
                if _dbg < 3:
                    bg.free = [mg]
                    bb.free = [mb]
                    fin = norm_ev[-1]
                    continue
                for tt in range(4):
                    l1 = P.op("vector", lambda e, tt=tt, blk=blk, bg=bg: e.tensor_tensor(
                        out=ACC[:, tt, blk * 512:(blk + 1) * 512], in0=ACC[:, tt, blk * 512:(blk + 1) * 512], in1=bg.ap, op=ALU.mult),
                        deps=[mg, norm_ev[tt]])
                    l2 = P.op("vector", lambda e, tt=tt, blk=blk, bb=bb: e.tensor_tensor(
                        out=ACC[:, tt, blk * 512:(blk + 1) * 512], in0=ACC[:, tt, blk * 512:(blk + 1) * 512], in1=bb.ap, op=ALU.add),
                        deps=[mb, l1])
                    fin = l2
                bg.free = [l1]
                bb.free = [l2]
            for tt in range(4):
                acc_b[tt].ready = [fin]
                acc_b[tt].free = []
            vb_b.free = [mg, mb]

        def wout_phase(w_out_l, ut_ready):
            fin = None
            mm = None
            wsrc = w_out_l.rearrange("(k p) n -> p k n", p=128)
            nkh = KC // 2
            for cbk in range(D // 512):
                banks = [psget() for _ in range(4)]
                for half in range(2):
                    wb, wv = wload(wsrc[:, half * nkh:(half + 1) * nkh, cbk * 512:(cbk + 1) * 512], nkh, 512)
                    for tt in range(4):
                        pb = banks[tt]
                        for k in range(nkh):
                            kc = half * nkh + k
                            mm = P.op("tensor", lambda e, tt=tt, kc=kc, k=k, pb=pb, wv=wv: e.matmul(
                                pb.ap[:, 0:512], lhsT=UT3[:, kc, tt * 128:(tt + 1) * 128], rhs=wv[:, k, :],
                                start=(kc == 0), stop=(kc == KC - 1)),
                                deps=(wb.ready + (pb.free if half == 0 else []) + ut_ready) if k == 0 else (), signal=(k == nkh - 1))
                    wb.free = [mm]
                for tt in range(4):
                    pb = banks[tt]
                    ev = P.op("vector", lambda e, tt=tt, cbk=cbk, pb=pb: e.scalar_tensor_tensor(
                        out=ACC[:, tt, cbk * 512:(cbk + 1) * 512], in0=ACC[:, tt, cbk * 512:(cbk + 1) * 512], scalar=c.ALPHA,
                        in1=pb.ap[:, 0:512], op0=ALU.mult, op1=ALU.add), deps=[mm] + acc_b[tt].free + acc_b[tt].ready)
                    pb.free = [ev]
                    fin = ev
            for tt in range(4):
                acc_b[tt].ready = [fin]
                acc_b[tt].free = []
            ut_b.free = [mm]

        def mixer_a(l):
            w_in = a_w_in[l].rearrange("(k p) n -> p k n", p=128)
            V3 = VBb.rearrange("p (t d) -> p t d", t=4)[:, :, 0:D]
            u_last = None
            mm = None
            for cbk in range(D // 256):
                wb, wv = wload(w_in[:, :, cbk * 256:(cbk + 1) * 256], KC, 256)
                for hf in range(2):
                    j = cbk * 2 + hf
                    pb = psget()
                    for kc in range(KC):
                        mm = P.op("tensor", lambda e, kc=kc, hf=hf, pb=pb, wv=wv: e.matmul(
                            pb.ap[:, 0:TT], lhsT=wv[:, kc, hf * 128:(hf + 1) * 128], rhs=XT3[:, kc, :],
                            start=(kc == 0), stop=(kc == KC - 1)),
                            deps=(wb.ready + pb.free + xt_b.ready) if kc == 0 else (), signal=(kc == KC - 1))
                    ev = P.op("scalar", lambda e, j=j, pb=pb: e.activation(out=UT3[:, j, :], in_=pb.ap[:, 0:TT], func=AF.Gelu_apprx_tanh),
                              deps=[mm] + ut_b.free)
                    pb.free = [ev]
                    u_last = ev
                wb.free = [mm]
            ut_b.free = []
            nvb = D // 512
            st_ev = [None] * 4
            nkh = KC // 2
            for cbk in range(nvb):
                banks = [psget() for _ in range(4)]
                for half in range(2):
                    wb, wv = wload(w_in[:, half * nkh:(half + 1) * nkh, D + cbk * 512:D + (cbk + 1) * 512], nkh, 512)
                    for tt in range(4):
                        pb = banks[tt]
                        for k in range(nkh):
                            kc = half * nkh + k
                            mm = P.op("tensor", lambda e, tt=tt, kc=kc, k=k, pb=pb, wv=wv: e.matmul(
                                pb.ap[:, 0:512], lhsT=XT3[:, kc, tt * 128:(tt + 1) * 128], rhs=wv[:, k, :],
                                start=(kc == 0), stop=(kc == KC - 1)),
                                deps=(wb.ready + (pb.free if half == 0 else []) + xt_b.ready) if k == 0 else (), signal=(k == nkh - 1))
                    wb.free = [mm]
                for tt in range(4):
                    pb = banks[tt]
                    ev = P.op("scalar", lambda e, tt=tt, cbk=cbk, pb=pb: e.activation(
                        out=V3[:, tt, cbk * 512:(cbk + 1) * 512], in_=pb.ap[:, 0:512], func=AF.Gelu_apprx_tanh),
                        deps=[mm] + vb_b.free)
                    pb.free = [ev]
                    st_ev[tt] = P.op("vector", lambda e, tt=tt, cbk=cbk: e.bn_stats(
                        out=lnst[:, tt * 96 + cbk * 6:tt * 96 + (cbk + 1) * 6], in_=V3[:, tt, cbk * 512:(cbk + 1) * 512]), deps=[ev])
            last_mm = mm
            vb_b.free = []
            o2 = c.AH * 128
            WmT = XT[:, 0:o2].rearrange("p (h t) -> p h t", h=c.AH)
            BT = XT[:, o2:o2 + 2 * KC * 128].bitcast(F32).rearrange("p (j t) -> p j t", j=KC)
            p1 = P.dma("sync", lambda e: e.dma_start(out=XT[:, 0:o2], in_=wmt_d[l]), sem_p, deps=[last_mm] + allconst)
            p2 = P.dma("sync", lambda e: e.dma_start(out=XT[:, o2:o2 + 2 * KC * 128].bitcast(F32), in_=bt_d[l]), sem_p,
                       deps=[last_mm] + allconst)
            g_last = None
            sp_last = None
            for tt in range(4):
                sv = lnst[:, tt * 96:tt * 96 + nvb * 6]
                mv = small[:, tt * 4:tt * 4 + 2]
                rs = small[:, tt * 4 + 2:tt * 4 + 3]
                a1 = P.op("vector", lambda e, sv=sv, mv=mv: e.bn_aggr(out=mv, in_=sv), deps=[st_ev[tt]])
                a2a = P.op("scalar", lambda e, mv=mv, rs=rs: e.activation(out=rs, in_=mv[:, 1:2], func=AF.Sqrt, bias=epsc[:, 0:1]), deps=[a1] + allconst)
                a2 = P.op("vector", lambda e, rs=rs: e.reciprocal(out=rs, in_=rs), deps=[a2a])
                a3 = P.op("vector", lambda e, tt=tt, mv=mv, rs=rs: e.tensor_scalar(
                    out=V3[:, tt, :], in0=V3[:, tt, :], scalar1=mv[:, 0:1], scalar2=rs, op0=ALU.subtract, op1=ALU.mult), deps=[a2])
                for jq in range(KC // 4):
                    pb = psget()
                    mm = None
                    for jj in range(4):
                        j = jq * 4 + jj
                        mm = P.op("tensor", lambda e, tt=tt, j=j, jj=jj, pb=pb: e.matmul(
                            pb.ap[:, jj * 128:(jj + 1) * 128], lhsT=V3[:, tt, j * 128:(j + 1) * 128], rhs=WmT[:, j // 2, :],
                            start=True, stop=True), deps=([a3, p1, p2] + pb.free) if jj == 0 else (), signal=(jj == 3))
                    sp_last = mm
                    ev2 = None
                    for jj in range(4):
                        j = jq * 4 + jj
                        tb = sp_tmp.get()
                        ev1 = P.op("vector", lambda e, j=j, jj=jj, pb=pb, tb=tb: e.scalar_tensor_tensor(
                            out=tb.ap, in0=pb.ap[:, jj * 128:(jj + 1) * 128], scalar=lng[:, l * KC + j:l * KC + j + 1],
                            in1=BT[:, j, :], op0=ALU.mult, op1=ALU.add), deps=[mm, p1, p2] + tb.free)
                        ev2 = P.op("vector", lambda e, tt=tt, j=j, tb=tb: e.tensor_tensor(
                            out=UT3[:, j, tt * 128:(tt + 1) * 128], in0=tb.ap, in1=UT3[:, j, tt * 128:(tt + 1) * 128], op=ALU.mult),
                            deps=[ev1, u_last])
                        tb.free = [ev2]
                        g_last = ev2
                    pb.free = [ev2]
            xt_b.free = [sp_last, g_last]
            vb_b.free = [sp_last]
            return [g_last]

        def mixer_b(l):
            w_in = b_w_in[l].rearrange("(k p) n -> p k n", p=128)
            g_last = None
            last_pe = None
            for h in range(c.BH):
                par = h % 2
                base = par * 3600
                xr = [VB[:, base + q * 515:base + (q + 1) * 515] for q in range(2)]
                xc = [VB[:, base + 1030 + q * 512:base + 1030 + (q + 1) * 512] for q in range(2)]
                tb = [VB[:, base + 2054 + q * 512:base + 2054 + (q + 1) * 512] for q in range(2)]
                xcb = VBb[:, 2 * (base + 3078):2 * (base + 3078) + 1024]
                ta = [xr[q][:, 0:512] for q in range(2)]
                wby, wvy = wload(w_in[:, :, h * 256:(h + 1) * 256], KC, 256)
                mm = None
                yev = []
                for hf in range(2):
                    j = h * 2 + hf
                    pb = psget()
                    for kc in range(KC):
                        mm = P.op("tensor", lambda e, kc=kc, hf=hf, pb=pb, wvy=wvy: e.matmul(
                            pb.ap[:, 0:TT], lhsT=wvy[:, kc, hf * 128:(hf + 1) * 128], rhs=XT3[:, kc, :],
                            start=(kc == 0), stop=(kc == KC - 1)),
                            deps=(wby.ready + pb.free + xt_b.ready) if kc == 0 else (), signal=(kc == KC - 1))
                    ev = P.op("scalar", lambda e, j=j, pb=pb: e.activation(out=UT3[:, j, :], in_=pb.ap[:, 0:TT], func=AF.Gelu_apprx_tanh),
                              deps=[mm] + ut_b.free)
                    pb.free = [ev]
                    yev.append(ev)
                wby.free = [mm]
                wbx, wvx = wload(w_in[:, :, D + h * 256:D + (h + 1) * 256], KC, 256)
                xev = []
                for hf in range(2):
                    j = h * 2 + hf
                    bj = l * KC + j
                    pb = psget()
                    for kc in range(KC):
                        mm = P.op("tensor", lambda e, kc=kc, hf=hf, pb=pb, wvx=wvx: e.matmul(
                            pb.ap[:, 0:TT], lhsT=wvx[:, kc, hf * 128:(hf + 1) * 128], rhs=XT3[:, kc, :],
                            start=(kc == 0), stop=(kc == KC - 1)),
                            deps=(wbx.ready + pb.free + xt_b.ready) if kc == 0 else (), signal=(kc == KC - 1))
                    hl = halo[:, bj * 3:bj * 3 + 3]
                    e0 = P.op("scalar", lambda e, hf=hf, hl=hl, xr=xr: e.copy(out=xr[hf][:, 0:3], in_=hl),
                              deps=vbfree[par] + vb_b.free + allconst)
                    e1_ = P.op("scalar", lambda e, hf=hf, pb=pb, xr=xr: e.copy(out=xr[hf][:, 3:515], in_=pb.ap[:, 0:TT]), deps=[mm, e0])
                    e2_ = P.op("scalar", lambda e, hf=hf, hl=hl, xr=xr: e.copy(out=hl, in_=xr[hf][:, 512:515]), deps=[e1_])
                    pb.free = [e1_]

                    def cwl(k, j=j):
                        o = (l * 4 + k) * KC + j
                        return cw[:, o:o + 1]
                    vk = P.op("vector", lambda e, hf=hf, bj=bj, xr=xr, xc=xc, cwl=cwl: e.tensor_scalar(
                        out=xc[hf], in0=xr[hf][:, 0:512], scalar1=cwl(0), scalar2=cb_[:, bj:bj + 1],
                        op0=ALU.mult, op1=ALU.add), deps=[e1_, e2_])
                    for k in range(1, 4):
                        vk = P.op("vector", lambda e, hf=hf, k=k, xr=xr, xc=xc, cwl=cwl: e.scalar_tensor_tensor(
                            out=xc[hf], in0=xr[hf][:, k:k + 512], scalar=cwl(k), in1=xc[hf], op0=ALU.mult, op1=ALU.add), deps=[vk])
                    xev.append(vk)
                wbx.free = [mm]
                cbev = []
                for hf in range(2):
                    cbev.append(P.op("scalar", lambda e, hf=hf, xc=xc, xcb=xcb: e.copy(out=xcb[:, hf * 512:(hf + 1) * 512], in_=xc[hf]),
                                     deps=[xev[hf]]))
                gwb = gw_r.get()
                gwr = gwb.ap[:, 0:512].rearrange("p (k n) -> p k n", k=2)
                gwi = gwb.ap[:, 512:1024].rearrange("p (k n) -> p k n", k=2)
                g1 = P.dma("gpsimd", lambda e, gwr=gwr, h=h: e.dma_start(out=gwr, in_=b_w_r[l, h].rearrange("(k p) n -> p k n", p=128)),
                           gwb.sem, deps=gwb.free)
                g2_ = P.dma("gpsimd", lambda e, gwi=gwi, h=h: e.dma_start(out=gwi, in_=b_w_i[l, h].rearrange("(k p) n -> p k n", p=128)),
                            gwb.sem, deps=gwb.free)
                gmm = None
                for hf in range(2):
                    j = h * 2 + hf
                    bj = l * KC + j
                    pr = psget()
                    pi = psget()
                    for (pp, gw) in ((pr, gwr), (pi, gwi)):
                        for k2 in range(2):
                            gmm = P.op("tensor", lambda e, pp=pp, gw=gw, k2=k2, hf=hf, xcb=xcb: e.matmul(
                                pp.ap[:, 0:TT], lhsT=gw[:, k2, hf * 128:(hf + 1) * 128], rhs=xcb[:, k2 * 512:(k2 + 1) * 512],
                                start=(k2 == 0), stop=(k2 == 1)),
                                deps=([g1, g2_] + cbev + pp.free) if k2 == 0 else (), signal=(k2 == 1))
                    r1 = P.op("scalar", lambda e, hf=hf, pr=pr, bj=bj, ta=ta: e.activation(out=ta[hf], in_=pr.ap[:, 0:TT], func=AF.Sigmoid,
                                                                                       bias=bbr[:, bj:bj + 1]), deps=[gmm] + xev)
                    i1 = P.op("scalar", lambda e, hf=hf, pi=pi, bj=bj, tb=tb: e.activation(out=tb[hf], in_=pi.ap[:, 0:TT], func=AF.Sigmoid,
                                                                                       bias=bbi[:, bj:bj + 1]), deps=[gmm])
                    pr.free = [r1]
                    pi.free = [i1]
                    a1 = P.op("scalar", lambda e, hf=hf, bj=bj, ta=ta: e.activation(out=ta[hf], in_=ta[hf], func=AF.Exp, scale=c8[:, bj:bj + 1]),
                              deps=[r1])
                    b1 = P.op("vector", lambda e, hf=hf, tb=tb, xc=xc: e.tensor_tensor(out=tb[hf], in0=tb[hf], in1=xc[hf], op=ALU.mult),
                              deps=[i1] + cbev)
                    m1 = P.op("vector", lambda e, hf=hf, ta=ta, xc=xc: e.scalar_tensor_tensor(out=xc[hf], in0=ta[hf], scalar=-1.0, in1=ta[hf],
                                                                                          op0=ALU.mult, op1=ALU.mult), deps=[a1, b1])
                    m2 = P.op("vector", lambda e, hf=hf, xc=xc: e.tensor_scalar(out=xc[hf], in0=xc[hf], scalar1=1.0, scalar2=1e-20,
                                                                            op0=ALU.add, op1=ALU.max), deps=[m1])
                    m3 = P.op("scalar", lambda e, hf=hf, xc=xc: e.activation(out=xc[hf], in_=xc[hf], func=AF.Sqrt), deps=[m2])
                    b2 = P.op("vector", lambda e, hf=hf, tb=tb, xc=xc: e.tensor_tensor(out=tb[hf], in0=tb[hf], in1=xc[hf], op=ALU.mult), deps=[m3])
                    hs = hstate[:, bj:bj + 1]
                    s1 = P.op("vector", lambda e, hf=hf, ta=ta, tb=tb, xc=xc, hs=hs: e.tensor_tensor_scan(
                        out=xc[hf], data0=ta[hf], data1=tb[hf], initial=hs, op0=ALU.mult, op1=ALU.add), deps=[b2] + allconst)
                    s2 = P.op("vector", lambda e, hf=hf, xc=xc, hs=hs: e.tensor_copy(out=hs, in_=xc[hf][:, 511:512]), deps=[s1])
                    s3 = P.op("vector", lambda e, hf=hf, j=j, xc=xc: e.tensor_tensor(out=UT3[:, j, :], in0=xc[hf], in1=UT3[:, j, :], op=ALU.mult),
                              deps=[s2, yev[hf]])
                    g_last = s3
                gwb.free = [gmm]
                vbfree[par] = [g_last, gmm]
                last_pe = gmm
            xt_b.free = [last_pe]
            vb_b.free = [g_last, last_pe]
            vbfree[0] = []
            vbfree[1] = []
            return [g_last]

        def moe(L, rd):
            EG, NG = c.EG, c.NG
            lg = VB[:, 3072:3072 + 4 * E].rearrange("p (t e) -> p t e", t=4)
            rb = VB[:, 3072 + 4 * E:3072 + 5 * E]
            exs = VB[:, 3072 + 5 * E:3072 + 9 * E]
            GT = VB[0:E, 3584:3584 + TT]
            bgu = VB[:, 4096:4096 + E * 2 * FC].rearrange("p (e k) -> p e k", e=E)
            bdn = [VB[0:E, q * 512:(q + 1) * 512] for q in range(2)]
            bdn_free = [[], []]
            tmp = [[VB[:, 5120 + (pz * 3 + q) * 512:5120 + (pz * 3 + q + 1) * 512] for q in range(3)] for pz in range(2)]
            tmp_free = [list(vb_b.free), list(vb_b.free)]
            r0 = P.dma("sync", lambda e: e.dma_start(out=rb, in_=router_b[L].partition_broadcast(128)), sem_p3, deps=vb_b.free)
            r1 = P.dma("sync", lambda e: e.dma_start(out=VB[:, 4096:4096 + E * 2 * FC], in_=l_b_gu[L]), sem_p3, deps=vb_b.free)
            gt_ev = []
            lgfree = []
            for tt in range(4):
                lt = lg[:, tt, :]
                m8 = small[:, 16 + tt * 8:16 + tt * 8 + 8]
                q1 = P.op("vector", lambda e, tt=tt, lt=lt: e.tensor_tensor(out=lt, in0=lgbank.ap[:, tt * E:(tt + 1) * E], in1=rb, op=ALU.add),
                          deps=[r0, r1] + rd)
                lgfree.append(q1)
                q2 = P.op("vector", lambda e, lt=lt, m8=m8: e.max(out=m8, in_=lt), deps=[q1])
                nm = small[:, 48 + tt:48 + tt + 1]
                q3 = P.op("vector", lambda e, m8=m8, nm=nm: e.tensor_scalar(out=nm, in0=m8[:, 0:1], scalar1=-1.0, scalar2=None, op0=ALU.mult),
                          deps=[q2])
                ex = exs[:, tt * E:(tt + 1) * E]
                q4 = P.op("scalar", lambda e, lt=lt, ex=ex, nm=nm: e.activation(out=ex, in_=lt, func=AF.Exp, bias=nm), deps=[q3])
                q5 = P.op("vector", lambda e, lt=lt, m8=m8: e.tensor_scalar(out=lt, in0=lt, scalar1=m8[:, c.TOPK - 1:c.TOPK], scalar2=None,
                                                                        op0=ALU.is_ge), deps=[q4])
                sm = small[:, 52 + tt:52 + tt + 1]
                q6 = P.op("vector", lambda e, lt=lt, ex=ex: e.tensor_tensor(out=lt, in0=lt, in1=ex, op=ALU.mult), deps=[q5])
                q7 = P.op("vector", lambda e, lt=lt, sm=sm: e.reduce_sum(out=sm, in_=lt, axis=AX.X), deps=[q6])
                q8 = P.op("vector", lambda e, sm=sm: e.reciprocal(out=sm, in_=sm), deps=[q7])
                q9 = P.op("vector", lambda e, lt=lt, sm=sm: e.tensor_scalar(out=lt, in0=lt, scalar1=sm, scalar2=None, op0=ALU.mult), deps=[q8])
                pb = psget()
                t1 = P.op("tensor", lambda e, lt=lt, pb=pb: e.transpose(out=pb.ap[0:E, 0:128], in_=lt, identity=ident[:]), deps=[q9] + pb.free)
                t2 = P.op("scalar", lambda e, tt=tt, pb=pb: e.copy(out=GT[:, tt * 128:(tt + 1) * 128], in_=pb.ap[0:E, 0:128]), deps=[t1])
                pb.free = [t2]
                gt_ev.append(t2)
                s0 = P.op("scalar", lambda e, tt=tt: e.mul(out=ACC[:, tt, :], in_=ACC[:, tt, :], mul=c.ALPHA), deps=rd + acc_b[tt].ready)
                acc_b[tt].ready = [s0]
            lgbank.free = lgfree
            H3 = UT[:, 0:EG * FC * TT].rearrange("p (k t) -> p k t", k=EG * FC)
            hdn_free = list(ut_b.free)
            fin = None
            pz = 0
            dmm = None
            wd = ex_w_down[L].rearrange("e (k p) n -> p (e k) n", p=128)
            nk = EG * FC
            for g in range(NG):
                h_last = None
                for ei in range(EG):
                    e_ = g * EG + ei
                    wsrc = ex_w_gu[L][e_].rearrange("(k p) n -> p k n", p=128)
                    banks = {}
                    for cbk in range(FC):
                        wb, wv = wload(wsrc[:, :, cbk * 256:(cbk + 1) * 256], KC, 256)
                        mm = None
                        for hf in range(2):
                            ch = cbk * 2 + hf
                            pb = psget()
                            for kc in range(KC):
                                mm = P.op("tensor", lambda e, kc=kc, hf=hf, pb=pb, wv=wv: e.matmul(
                                    pb.ap[:, 0:TT], lhsT=wv[:, kc, hf * 128:(hf + 1) * 128], rhs=XT3[:, kc, :],
                                    start=(kc == 0), stop=(kc == KC - 1)),
                                    deps=(wb.ready + pb.free + xt_b.ready) if kc == 0 else (), signal=(kc == KC - 1))
                            banks[ch] = (pb, mm)
                        wb.free = [mm]
                    gb = psget()
                    sel = ident[0:E, e_:e_ + 1].to_broadcast([E, 128])
                    gm = P.op("tensor", lambda e, gb=gb, sel=sel: e.matmul(gb.ap[:, 0:TT], lhsT=sel, rhs=GT, start=True, stop=True),
                              deps=gt_ev + gb.free + allconst)
                    for cc in range(FC):
                        pg_, mg_ = banks[cc]
                        pu_, mu_ = banks[FC + cc]
                        t0, t1_, t2_ = tmp[pz]
                        w0 = P.op("vector", lambda e, pg_=pg_, t0=t0, e_=e_, cc=cc: e.tensor_scalar(
                            out=t0, in0=pg_.ap[:, 0:TT], scalar1=bgu[:, e_, cc:cc + 1], scalar2=7.0, op0=ALU.add, op1=ALU.min),
                            deps=[mg_, r0, r1] + tmp_free[pz])
                        pg_.free = [w0]
                        w1 = P.op("scalar", lambda e, t0=t0, t1_=t1_: e.activation(out=t1_, in_=t0, func=AF.Sigmoid, scale=1.702), deps=[w0])
                        w2 = P.op("vector", lambda e, pu_=pu_, t2_=t2_, e_=e_, cc=cc: e.tensor_scalar(
                            out=t2_, in0=pu_.ap[:, 0:TT], scalar1=bgu[:, e_, FC + cc:FC + cc + 1], scalar2=7.0, op0=ALU.add, op1=ALU.min),
                            deps=[mu_, w0])
                        pu_.free = [w2]
                        w3 = P.op("vector", lambda e, t2_=t2_: e.tensor_scalar(out=t2_, in0=t2_, scalar1=-7.0, scalar2=1.0, op0=ALU.max, op1=ALU.add),
                                  deps=[w2])
                        w4 = P.op("vector", lambda e, t0=t0, t1_=t1_: e.tensor_tensor(out=t0, in0=t0, in1=t1_, op=ALU.mult), deps=[w1, w3])
                        w5 = P.op("vector", lambda e, t0=t0, t2_=t2_: e.tensor_tensor(out=t0, in0=t0, in1=t2_, op=ALU.mult), deps=[w4])
                        w6 = P.op("vector", lambda e, t0=t0, gb=gb, ei=ei, cc=cc: e.tensor_tensor(
                            out=H3[:, ei * FC + cc, :], in0=t0, in1=gb.ap[:, 0:TT], op=ALU.mult), deps=[w5, gm] + hdn_free)
                        tmp_free[pz] = [w6]
                        pz ^= 1
                        h_last = w6
                    gb.free = [h_last]
                hdn_free = []
                nkh2 = nk // 2
                for cbk in range(D // 512):
                    bq = cbk % 2
                    bl = None
                    if g == 0:
                        bl = P.dma("sync", lambda e, bq=bq, cbk=cbk: e.dma_start(out=bdn[bq], in_=ex_b_down[L][:, cbk * 512:(cbk + 1) * 512]),
                                   bsem[bq], deps=bdn_free[bq] + [r0, r1] + rd)
                    banks = [psget() for _ in range(4)]
                    for half in range(2):
                        wb, wv = wload(wd[:, g * nk + half * nkh2:g * nk + (half + 1) * nkh2, cbk * 512:(cbk + 1) * 512], nkh2, 512)
                        for tt in range(4):
                            pb = banks[tt]
                            if g == 0 and half == 0:
                                P.op("tensor", lambda e, tt=tt, pb=pb, bq=bq: e.matmul(pb.ap[:, 0:512], lhsT=GT[:, tt * 128:(tt + 1) * 128], rhs=bdn[bq],
                                                                                       start=True, stop=False),
                                     deps=[bl] + gt_ev + pb.free, signal=False)
                            for k in range(nkh2):
                                kk = half * nkh2 + k
                                dmm = P.op("tensor", lambda e, tt=tt, k=k, kk=kk, pb=pb, wv=wv, g=g: e.matmul(
                                    pb.ap[:, 0:512], lhsT=H3[:, kk, tt * 128:(tt + 1) * 128], rhs=wv[:, k, :],
                                    start=(kk == 0 and g != 0), stop=(kk == nk - 1)),
                                    deps=(wb.ready + [h_last] + (pb.free if (g != 0 and half == 0) else [])) if k == 0 else (),
                                    signal=(k == nkh2 - 1))
                        wb.free = [dmm]
                    for tt in range(4):
                        pb = banks[tt]
                        ev = P.op("vector", lambda e, tt=tt, cbk=cbk, pb=pb: e.tensor_tensor(
                            out=ACC[:, tt, cbk * 512:(cbk + 1) * 512], in0=ACC[:, tt, cbk * 512:(cbk + 1) * 512], in1=pb.ap[:, 0:512], op=ALU.add),
                            deps=[dmm] + acc_b[tt].ready)
                        pb.free = [ev]
                        fin = ev
                    if g == 0:
                        bdn_free[bq] = [dmm]
                hdn_free = [dmm]
            ut_b.free = [dmm]
            xt_b.free = [dmm]
            for tt in range(4):
                acc_b[tt].ready = [fin]
                acc_b[tt].free = []
            vb_b.free = [dmm, fin]

        out_ev = []
        for s in range(NST):
            for tt in range(4):
                r0_ = s * TT + tt * 128
                ev = P.dma("sync", lambda e, tt=tt, r0_=r0_: e.dma_start(out=ACC[:, tt, :], in_=x[r0_:r0_ + 128, :]), sem_in[tt],
                           deps=acc_b[tt].free + acc_b[tt].ready)
                acc_b[tt].ready = [ev]
                acc_b[tt].free = []
            for L in layers:
                l = L // 2
                cur_layer[0] = L
                if upto < 1:
                    break
                build_xt()
                if upto < 2:
                    break
                if L % 2 == 0:
                    ut_ready = mixer_a(l)
                else:
                    ut_ready = mixer_b(l)
                if upto < 3:
                    break
                wout_phase(a_w_out[l] if L % 2 == 0 else b_w_out[l], ut_ready)
                if upto < 4:
                    break
                layer_norm(ln1_g[L], ln1_b[L])
                if upto < 5:
                    break
                rd = build_xt(router_layer=L)
                if upto < 6:
                    break
                moe(L, rd)
                if upto < 7:
                    break
                layer_norm(ln2_g[L], ln2_b[L])
            for tt in range(4):
                r0_ = s * TT + tt * 128
                ev = P.dma("sync", lambda e, tt=tt, r0_=r0_: e.dma_start(out=out[r0_:r0_ + 128, :], in_=ACC[:, tt, :]), sem_out[tt],
                           deps=acc_b[tt].ready)
                acc_b[tt].free = [ev]
                out_ev.append(ev)
        P.wait_only("sync", out_ev)
        with nc.Block() as block:
            P.replay(block)
    return nc, P


def _pj(v, KC):
    v = np.asarray(v, np.float32)
    lead = int(np.prod(v.shape[:-1])) if v.ndim > 1 else 1
    v = v.reshape(lead, KC, 128)
    return np.ascontiguousarray(v.transpose(2, 0, 1).reshape(128, lead * KC))


def prep_inputs(cfg, inputs):
    c = cfg
    KC, E, FC = c.KC, c.E, c.FC
    f = lambda k: np.ascontiguousarray(inputs[k], dtype=np.float32)
    m = {}
    for k in ["a_w_in", "a_w_out", "b_w_in", "b_w_r", "b_w_i", "b_w_out", "ln1_g", "ln1_b", "ln2_g", "ln2_b",
              "router_b", "ex_w_gu", "ex_w_down", "ex_b_down", "a_b_s"]:
        m[k] = f(k)
    m["l_a_ln_g"] = _pj(f("a_ln_g"), KC)
    m["l_a_ln_b"] = _pj(f("a_ln_b"), KC)
    m["l_a_w_sT"] = np.ascontiguousarray(f("a_w_s").transpose(0, 3, 1, 2).reshape(c.nA, 128, c.AH * 128))
    m["l_conv_w"] = _pj(f("b_conv_w"), KC)
    m["l_conv_b"] = _pj(f("b_conv_b"), KC)
    m["l_b_r"] = _pj(f("b_b_r").reshape(c.nB, -1), KC)
    m["l_b_i"] = _pj(f("b_b_i").reshape(c.nB, -1), KC)
    m["l_lam"] = _pj(f("b_lambda"), KC)
    rw = f("router_w").reshape(c.DEPTH, KC, 128, E)
    m["l_router_w"] = np.ascontiguousarray(rw.transpose(0, 2, 1, 3).reshape(c.DEPTH, 128, KC * E))
    bg = f("ex_b_gu").reshape(c.DEPTH, E, 2 * FC, 128)
    m["l_b_gu"] = np.ascontiguousarray(bg.transpose(0, 3, 1, 2).reshape(c.DEPTH, 128, E * 2 * FC))
    m["c_ident"] = np.eye(128, dtype=np.float32)
    m["c_triu"] = np.triu(np.ones((128, 128), np.float32))
    return m


def run(cfg, inputs, layers=None, trace=False, upto=99):
    nc, P = build_program(cfg, layers, upto)
    x = np.ascontiguousarray(inputs["x"], dtype=np.float32)
    xs = x.reshape(-1, cfg.D)
    n = cfg.n_cores
    per = xs.shape[0] // n
    assert per == cfg.NTOK
    base = prep_inputs(cfg, inputs)
    in_maps = []
    for ci in range(n):
        m = dict(base)
        m["x"] = xs[ci * per:(ci + 1) * per]
        in_maps.append(m)
    res = run_bass_kernel_spmd(nc, in_maps, core_ids=list(range(n)), trace=trace)
    y = np.concatenate([r["out"] for r in res.results], axis=0).reshape(x.shape)
    return y, res


def kernel(**inputs):
    cfg = Cfg()
    y, _ = run(cfg, inputs)
    return y.astype(np.float32)
```
